# Optimizing a Trainium2 kernel written in Bass

```python
import math
import jax
import jax.numpy as jnp
from jax import lax
import numpy as np

D_MODEL = 1024
BATCH = 16
SEQ = 4096
DEPTH = 4

GRID_W = 64
CTX_LEN = 256
EPS = 1e-6
N_MOD = 6

DN_QK_HEADS = 8
DN_V_HEADS = 16
DN_HEAD_DIM = 128
DN_REP = DN_V_HEADS // DN_QK_HEADS
DN_KEY_W = DN_QK_HEADS * DN_HEAD_DIM
DN_VAL_W = DN_V_HEADS * DN_HEAD_DIM
DN_CONV_W = 2 * DN_KEY_W + DN_VAL_W
DN_IN_W = DN_CONV_W + DN_VAL_W + 4 * DN_V_HEADS
DN_CONV = 5
DN_CHUNK = 64

ATT_Q_HEADS = 8
ATT_KV_HEADS = 2
ATT_GROUP = ATT_Q_HEADS // ATT_KV_HEADS
ATT_HEAD_DIM = 128
ATT_Q_W = ATT_Q_HEADS * ATT_HEAD_DIM
ATT_KV_W = ATT_KV_HEADS * ATT_HEAD_DIM
ATT_IN_W = ATT_Q_W + 2 * ATT_KV_W
ATT_BLOCK = 128
ROPE_THETA = 10000.0

PEER_HEADS = 8
PEER_N_KEYS = 128
PEER_N_EXPERTS = PEER_N_KEYS * PEER_N_KEYS
PEER_QUERY_DIM = 256
PEER_HALF = PEER_QUERY_DIM // 2
PEER_TOPK = 16
PEER_TOKEN_BLOCK = 128

N_DN_LAYERS = (DEPTH + 1) // 2
N_ATT_LAYERS = DEPTH // 2

kernel_name = 'hybrid_deltanet_gqa_peer_dit'


def rmsnorm(x, gain):
    xf = x.astype(jnp.float32)
    y = xf * lax.rsqrt(jnp.mean(xf * xf, axis=-1, keepdims=True) + EPS)
    return (y * gain.astype(jnp.float32)).astype(x.dtype)


def l2norm(x):
    xf = x.astype(jnp.float32)
    return xf * lax.rsqrt(jnp.sum(xf * xf, axis=-1, keepdims=True) + EPS)


def ada_params(cond, w, b):
    return jnp.split((cond @ w + b)[:, None, :], N_MOD, axis=-1)


def axial_rope_tables(n_tokens):
    rows = n_tokens // GRID_W
    row = jnp.broadcast_to(jnp.arange(rows)[:, None], (rows, GRID_W)).reshape(-1).astype(jnp.float32)
    col = jnp.broadcast_to(jnp.arange(GRID_W)[None, :], (rows, GRID_W)).reshape(-1).astype(jnp.float32)
    axis_dim = ATT_HEAD_DIM // 2
    freqs = ROPE_THETA ** (-jnp.arange(0, axis_dim, 2, dtype=jnp.float32) / axis_dim)
    ang = jnp.concatenate([row[:, None] * freqs, col[:, None] * freqs], axis=-1)
    return jnp.cos(ang), jnp.sin(ang)


def apply_rope(x, cos, sin):
    xf = x.astype(jnp.float32).reshape(x.shape[:-1] + (x.shape[-1] // 2, 2))
    x1, x2 = xf[..., 0], xf[..., 1]
    c = cos[None, :, None, :]
    s = sin[None, :, None, :]
    return jnp.stack([x1 * c - x2 * s, x1 * s + x2 * c], axis=-1).reshape(x.shape).astype(x.dtype)


def centred_dwconv(x, w):
    p = w.shape[0] // 2
    return lax.conv_general_dilated(x, w[:, None, :].astype(x.dtype), window_strides=(1,), padding=[(p, p)],
                                    dimension_numbers=('NWC', 'WIO', 'NWC'), feature_group_count=x.shape[-1])


def gated_delta_chunked(q, k, v, g, beta, s0):
    B, T, H, _ = q.shape
    dv = v.shape[-1]
    n = T // DN_CHUNK

    def chunkify(a):
        a = a.reshape((B, n, DN_CHUNK, H) + a.shape[3:])
        return jnp.moveaxis(a, (1, 3), (0, 2))

    incl = jnp.tril(jnp.ones((DN_CHUNK, DN_CHUNK), dtype=bool))
    strict = jnp.tril(jnp.ones((DN_CHUNK, DN_CHUNK), dtype=bool), -1)
    eye = jnp.eye(DN_CHUNK, dtype=jnp.float32)

    def step(S, inp):
        qc, kc, vc, gc, bc = inp
        gc = jnp.cumsum(gc, axis=-1)
        decay = jnp.exp(jnp.where(incl, gc[..., :, None] - gc[..., None, :], -jnp.inf))
        kk = jnp.einsum('bhid,bhjd->bhij', kc, kc)
        a = eye + jnp.where(strict, kk * decay * bc[..., :, None], 0.0)
        rhs = jnp.concatenate([vc * bc[..., None], kc * (bc * jnp.exp(gc))[..., None]], axis=-1)
        sol = lax.linalg.triangular_solve(a, rhs, left_side=True, lower=True, unit_diagonal=True)
        u, w = sol[..., :dv], sol[..., dv:]
        v_new = u - jnp.einsum('bhcd,bhde->bhce', w, S)
        qk = jnp.where(incl, jnp.einsum('bhid,bhjd->bhij', qc, kc) * decay, 0.0)
        o = (jnp.einsum('bhcd,bhde->bhce', qc * jnp.exp(gc)[..., None], S)
             + jnp.einsum('bhij,bhje->bhie', qk, v_new))
        g_last = gc[..., -1:]
        S = (S * jnp.exp(g_last)[..., None]
             + jnp.einsum('bhcd,bhce->bhde', kc * jnp.exp(g_last - gc)[..., None], v_new))
        return S, o

    S, o = lax.scan(step, s0, (chunkify(q), chunkify(k), chunkify(v), chunkify(g), chunkify(beta)))
    return jnp.moveaxis(o, (0, 2), (1, 3)).reshape(B, T, H, dv), S


def deltanet_mixer(h_ctx, h_lat, w_in, conv_w, a_log, dt_bias, norm_g, w_out, need_ctx_out):
    def prep(h):
        B, T, _ = h.shape
        proj = h @ w_in
        qkv = jax.nn.silu(centred_dwconv(proj[..., :DN_CONV_W], conv_w))
        q = l2norm(qkv[..., :DN_KEY_W].reshape(B, T, DN_QK_HEADS, DN_HEAD_DIM))
        k = l2norm(qkv[..., DN_KEY_W:2 * DN_KEY_W].reshape(B, T, DN_QK_HEADS, DN_HEAD_DIM))
        v = qkv[..., 2 * DN_KEY_W:].reshape(B, T, DN_V_HEADS, DN_HEAD_DIM).astype(jnp.float32)
        q = jnp.repeat(q, DN_REP, axis=2) * (DN_HEAD_DIM ** -0.5)
        k = jnp.repeat(k, DN_REP, axis=2)
        z = proj[..., DN_CONV_W:DN_CONV_W + DN_VAL_W].reshape(B, T, DN_V_HEADS, DN_HEAD_DIM)
        gates = proj[..., DN_CONV_W + DN_VAL_W:].astype(jnp.float32).reshape(B, T, 2, 2, DN_V_HEADS)
        beta = jax.nn.sigmoid(gates[..., 0, :])
        g = -jnp.exp(a_log) * jax.nn.softplus(gates[..., 1, :] + dt_bias)
        return q, k, v, g, beta, z

    def bidir(q, k, v, g, beta, s_f, s_b):
        o_f, s_f = gated_delta_chunked(q, k, v, g[:, :, 0], beta[:, :, 0], s_f)
        fl = lambda a: jnp.flip(a, axis=1)
        o_b, s_b = gated_delta_chunked(fl(q), fl(k), fl(v), fl(g[:, :, 1]), fl(beta[:, :, 1]), s_b)
        return o_f + fl(o_b), s_f, s_b

    def out(o, z, dtype):
        B, T = o.shape[:2]
        o = rmsnorm(o, norm_g) * jax.nn.silu(z.astype(jnp.float32))
        return o.reshape(B, T, DN_VAL_W).astype(dtype) @ w_out

    qc, kc, vc, gc, bc, zc = prep(h_ctx)
    B = h_ctx.shape[0]
    s0 = jnp.zeros((B, DN_V_HEADS, DN_HEAD_DIM, DN_HEAD_DIM), jnp.float32)
    o_c, s_f, s_b = bidir(qc, kc, vc, gc, bc, s0, s0)
    ql, kl, vl, gl, bl, zl = prep(h_lat)
    o_l, _, _ = bidir(ql, kl, vl, gl, bl, s_f, s_b)
    y_c = out(o_c, zc, h_ctx.dtype) if need_ctx_out else None
    return y_c, out(o_l, zl, h_lat.dtype)


def gqa_attend(q, k, v):
    s = jnp.einsum('bqhgd,bkhd->bhgqk', q, k).astype(jnp.float32) * (ATT_HEAD_DIM ** -0.5)
    p = jax.nn.softmax(s, axis=-1).astype(v.dtype)
    return jnp.einsum('bhgqk,bkhd->bqhgd', p, v)


def attention_mixer(h_ctx, h_lat, w_in, qn_g, kn_g, w_out, cos, sin, need_ctx_out):
    def proj(h):
        B, T, _ = h.shape
        p = h @ w_in
        q = p[..., :ATT_Q_W].reshape(B, T, ATT_Q_HEADS, ATT_HEAD_DIM)
        k = rmsnorm(p[..., ATT_Q_W:ATT_Q_W + ATT_KV_W].reshape(B, T, ATT_KV_HEADS, ATT_HEAD_DIM), kn_g)
        v = p[..., ATT_Q_W + ATT_KV_W:].reshape(B, T, ATT_KV_HEADS, ATT_HEAD_DIM)
        return q, k, v

    q_c, k_c, v_c = proj(h_ctx)
    q_l, k_l, v_l = proj(h_lat)
    q_l = apply_rope(rmsnorm(q_l, qn_g), cos, sin)
    k_l = apply_rope(k_l, cos, sin)
    keys = jnp.concatenate([k_c, k_l], axis=1)
    vals = jnp.concatenate([v_c, v_l], axis=1)
    B, T = h_lat.shape[:2]
    nblk = T // ATT_BLOCK
    qb = q_l.reshape(B, nblk, ATT_BLOCK, ATT_KV_HEADS, ATT_GROUP, ATT_HEAD_DIM).swapaxes(0, 1)
    o_l = lax.map(lambda blk: gqa_attend(blk, keys, vals), qb)
    y_l = o_l.swapaxes(0, 1).reshape(B, T, ATT_Q_W) @ w_out
    y_c = None
    if need_ctx_out:
        Lc = h_ctx.shape[1]
        q_c = rmsnorm(q_c, qn_g).reshape(B, Lc, ATT_KV_HEADS, ATT_GROUP, ATT_HEAD_DIM)
        y_c = gqa_attend(q_c, k_c, v_c).reshape(B, Lc, ATT_Q_W) @ w_out
    return y_c, y_l


def peer(h, w_query, sub_keys, u_tab, v_tab):
    B, T, D = h.shape
    blocks = h.reshape(-1, PEER_TOKEN_BLOCK, D)

    def block_fn(xb):
        P = xb.shape[0]
        qry = (xb @ w_query).reshape(P, PEER_HEADS, 2, PEER_HALF)
        s = jnp.einsum('phcd,hckd->phck', qry, sub_keys).astype(jnp.float32)
        top_s, top_i = lax.top_k(s, PEER_TOPK)
        cand_s = (top_s[..., 0, :, None] + top_s[..., 1, None, :]).reshape(P, PEER_HEADS, PEER_TOPK * PEER_TOPK)
        cand_i = (top_i[..., 0, :, None] * PEER_N_KEYS + top_i[..., 1, None, :]).reshape(P, PEER_HEADS, PEER_TOPK * PEER_TOPK)
        best_s, pos = lax.top_k(cand_s, PEER_TOPK)
        idx = jnp.take_along_axis(cand_i, pos, axis=-1)
        gate = jax.nn.softmax(best_s, axis=-1)
        act = jax.nn.gelu(jnp.einsum('pd,phkd->phk', xb, u_tab[idx]).astype(jnp.float32), approximate=False)
        coef = (gate * act).astype(xb.dtype)
        return jnp.einsum('phk,phkd->pd', coef, v_tab[idx])

    return lax.map(block_fn, blocks).reshape(B, T, D)


def setup_inputs(seed: int = 0) -> dict:
    key = jax.random.key(seed)
    ks = jax.random.split(key, 24)
    f32 = jnp.float32

    def nrm(k, shape, scale):
        return jax.random.normal(k, shape, f32) * scale

    def gain(k, shape):
        return 1.0 + 0.02 * jax.random.normal(k, shape, f32)

    dt = jnp.exp(jax.random.uniform(ks[13], (N_DN_LAYERS, 2, DN_V_HEADS), f32, math.log(1e-3), math.log(1e-1)))
    return {
        'x': nrm(ks[0], (BATCH, SEQ, D_MODEL), 1.0),
        'c': nrm(ks[1], (BATCH, D_MODEL), 1.0),
        'ctx': nrm(ks[2], (BATCH, CTX_LEN, D_MODEL), 1.0),
        'c_ctx': nrm(ks[3], (D_MODEL,), 1.0),
        'ada_w': nrm(ks[4], (DEPTH, D_MODEL, N_MOD * D_MODEL), 0.5 * D_MODEL ** -0.5),
        'ada_b': nrm(ks[5], (DEPTH, N_MOD * D_MODEL), 0.02),
        'norm1_g': gain(ks[6], (DEPTH, D_MODEL)),
        'norm2_g': gain(ks[7], (DEPTH, D_MODEL)),
        'final_g': gain(ks[8], (D_MODEL,)),
        'dn_w_in': nrm(ks[9], (N_DN_LAYERS, D_MODEL, DN_IN_W), D_MODEL ** -0.5),
        'dn_conv_w': nrm(ks[10], (N_DN_LAYERS, DN_CONV, DN_CONV_W), DN_CONV ** -0.5),
        'dn_a_log': jnp.log(jax.random.uniform(ks[11], (N_DN_LAYERS, 2, DN_V_HEADS), f32, 1.0, 16.0)),
        'dn_dt_bias': dt + jnp.log(-jnp.expm1(-dt)),
        'dn_norm_g': gain(ks[12], (N_DN_LAYERS, DN_HEAD_DIM)),
        'dn_w_out': nrm(ks[14], (N_DN_LAYERS, DN_VAL_W, D_MODEL), DN_VAL_W ** -0.5),
        'att_w_in': nrm(ks[15], (N_ATT_LAYERS, D_MODEL, ATT_IN_W), D_MODEL ** -0.5),
        'att_qn_g': gain(ks[16], (N_ATT_LAYERS, ATT_HEAD_DIM)),
        'att_kn_g': gain(ks[17], (N_ATT_LAYERS, ATT_HEAD_DIM)),
        'att_w_out': nrm(ks[18], (N_ATT_LAYERS, ATT_Q_W, D_MODEL), ATT_Q_W ** -0.5),
        'peer_w_query': nrm(ks[19], (DEPTH, D_MODEL, PEER_HEADS * PEER_QUERY_DIM), D_MODEL ** -0.5),
        'peer_sub_keys': nrm(ks[20], (DEPTH, PEER_HEADS, 2, PEER_N_KEYS, PEER_HALF), PEER_HALF ** -0.5),
        'peer_u': nrm(ks[21], (DEPTH, PEER_N_EXPERTS, D_MODEL), D_MODEL ** -0.5),
        'peer_v': nrm(ks[22], (DEPTH, PEER_N_EXPERTS, D_MODEL), PEER_HEADS ** -0.5),
    }


def reference(x, c, ctx, c_ctx, ada_w, ada_b, norm1_g, norm2_g, final_g,
              dn_w_in, dn_conv_w, dn_a_log, dn_dt_bias, dn_norm_g, dn_w_out,
              att_w_in, att_qn_g, att_kn_g, att_w_out,
              peer_w_query, peer_sub_keys, peer_u, peer_v):
    cos, sin = axial_rope_tables(x.shape[1])
    cond_lat = jax.nn.silu(c)
    cond_ctx = jax.nn.silu(c_ctx)[None]
    xl, xc = x, ctx
    for i in range(DEPTH):
        last = i == DEPTH - 1
        sh1l, sc1l, g1l, sh2l, sc2l, g2l = ada_params(cond_lat, ada_w[i], ada_b[i])
        sh1c, sc1c, g1c, sh2c, sc2c, g2c = ada_params(cond_ctx, ada_w[i], ada_b[i])
        hl = rmsnorm(xl, norm1_g[i]) * (1.0 + sc1l) + sh1l
        hc = rmsnorm(xc, norm1_g[i]) * (1.0 + sc1c) + sh1c
        j = i // 2
        if i % 2 == 0:
            yc, yl = deltanet_mixer(hc, hl, dn_w_in[j], dn_conv_w[j], dn_a_log[j], dn_dt_bias[j],
                                    dn_norm_g[j], dn_w_out[j], not last)
        else:
            yc, yl = attention_mixer(hc, hl, att_w_in[j], att_qn_g[j], att_kn_g[j], att_w_out[j],
                                     cos, sin, not last)
        xl = xl + g1l * yl
        hl = rmsnorm(xl, norm2_g[i]) * (1.0 + sc2l) + sh2l
        xl = xl + g2l * peer(hl, peer_w_query[i], peer_sub_keys[i], peer_u[i], peer_v[i])
        if not last:
            xc = xc + g1c * yc
            hc = rmsnorm(xc, norm2_g[i]) * (1.0 + sc2c) + sh2c
            xc = xc + g2c * peer(hc, peer_w_query[i], peer_sub_keys[i], peer_u[i], peer_v[i])
    return rmsnorm(xl, final_g)
```

```python
import contextlib
import numpy as np
import concourse.bass as bass
import concourse.mybir as mybir
from concourse.bass_utils import run_bass_kernel_spmd

F32 = mybir.dt.float32
BF16 = mybir.dt.bfloat16
I32 = mybir.dt.int32
U32 = mybir.dt.uint32
AF = mybir.ActivationFunctionType
ALU = mybir.AluOpType
AX = mybir.AxisListType

D = 1024
EPS = 1e-6
NCORES = 8


class Buf:
    __slots__ = ("w", "r", "name")

    def __init__(self, name=""):
        self.w = None
        self.r = {}
        self.name = name


class TK:
    def __init__(self, nc, es, n_dma_sems=40):
        self.nc = nc
        self.engs = {"pe": nc.tensor, "act": nc.scalar, "dve": nc.vector, "pool": nc.gpsimd, "sp": nc.sync}
        self.sem = {k: es.enter_context(nc.semaphore("sem_" + k)) for k in self.engs}
        self.cnt = {k: 0 for k in self.engs}
        self.seen = {k: {} for k in self.engs}
        self.dsems = [es.enter_context(nc.semaphore("dsem%d" % i)) for i in range(n_dma_sems)]
        self.dissued = [0] * n_dma_sems
        self.drr = 0
        self.ninst = 0
        self.outst = {k: [] for k in self.engs}
        self.max_out = 6

    def _wait(self, eng, key, val):
        if key == ("E", "pe") and eng == "pe":
            return
        if self.seen[eng].get(key, 0) >= val:
            return
        if key[0] == "D":
            val = max(val, self.dissued[key[1]])
            sem = self.dsems[key[1]]
        else:
            sem = self.sem[key[1]]
        self.engs[eng].wait_ge(sem, val)
        self.seen[eng][key] = val
        self.ninst += 1

    def _deps(self, eng, r, w):
        for b in r:
            if b.w is not None:
                self._wait(eng, b.w[0], b.w[1])
        for b in w:
            if b.w is not None:
                self._wait(eng, b.w[0], b.w[1])
            for k, v in b.r.items():
                self._wait(eng, k, v)

    def _commit(self, key, val, r, w):
        for b in r:
            if b.r.get(key, 0) < val:
                b.r[key] = val
        for b in w:
            b.w = (key, val)
            b.r = {}

    def op(self, eng, r, w, meth, *a, **kw):
        self._deps(eng, r, w)
        inst = getattr(self.engs[eng], meth)(*a, **kw)
        inst.then_inc(self.sem[eng], 1)
        self.cnt[eng] += 1
        self.ninst += 1
        self._commit(("E", eng), self.cnt[eng], r, w)
        return inst

    def dma(self, q, r, w, out, in_, indirect=None, **kw):
        self._deps(q, r, w)
        fifo = self.outst[q]
        while len(fifo) >= self.max_out:
            k0, v0 = fifo.pop(0)
            self._wait(q, k0, v0)
        i = self.drr
        self.drr = (self.drr + 1) % len(self.dsems)
        if indirect is None:
            inst = self.engs[q].dma_start(out=out, in_=in_, **kw)
        else:
            inst = self.engs[q].indirect_dma_start(out=out, out_offset=None, in_=in_, in_offset=indirect, **kw)
        inst.then_inc(self.dsems[i], 16)
        self.dissued[i] += 16
        self.ninst += 1
        self._commit(("D", i), self.dissued[i], r, w)
        fifo.append((("D", i), self.dissued[i]))
        return inst

    def barrier(self):
        for e in self.engs:
            for o in self.engs:
                if o != e and self.cnt[o] > 0:
                    self._wait(e, ("E", o), self.cnt[o])
            for i, v in enumerate(self.dissued):
                if v > 0:
                    self._wait(e, ("D", i), v)


def bc_ap(base, dims):
    a = base.ap
    return bass.AP(base.tensor, base.offset, [list(a[0])] + [list(d) for d in dims])


class Cfg:
    def __init__(self, NB=2, T=4096, CL=256, layers=("dn", "att", "dn", "att"), peer=True):
        self.NB, self.T, self.CL, self.layers, self.peer = NB, T, CL, tuple(layers), peer
        self.NT = T + CL


def build_program(cfg):
    NB, T, CL, NT = cfg.NB, cfg.T, cfg.CL, cfg.NT
    depth = len(cfg.layers)
    n_dn = max(1, sum(1 for m in cfg.layers if m == "dn"))
    n_att = max(1, sum(1 for m in cfg.layers if m == "att"))
    nc = bass.Bass("TRN2", target_bir_lowering=False)

    in_names = []
    has_dn = "dn" in cfg.layers
    has_att = "att" in cfg.layers

    def din(name, shape, dt=F32):
        if (name.startswith("dn_") or name == "c_masks") and not has_dn:
            return None
        if (name.startswith("att_") or name == "c_rope") and not has_att:
            return None
        if (name.startswith("peer_") or name == "c_iota") and not cfg.peer:
            return None
        if name in ("cnd", "ada_w", "ada_b", "norm1_g", "norm2_g") and not (has_dn or has_att or cfg.peer):
            return None
        in_names.append(name)
        return nc.dram_tensor(name, list(shape), dt, kind="ExternalInput").ap()

    def dscr(name, shape, dt=F32):
        if name == "MOD" and not (has_dn or has_att or cfg.peer):
            return None
        if name in ("QKT", "VV", "ZZ", "GB", "OO") and not has_dn:
            return None
        if name in ("AQT", "AKT", "AVV", "AO") and not has_att:
            return None
        return nc.dram_tensor(name, list(shape), dt, kind="Internal").ap()

    xin = din("xin", [NB, NT, D])
    cnd = din("cnd", [3, D])
    ada_w = din("ada_w", [depth, D, 6 * D])
    ada_b = din("ada_b", [depth, 6 * D])
    norm1_g = din("norm1_g", [depth, D])
    norm2_g = din("norm2_g", [depth, D])
    final_g = din("final_g", [1, D])
    dn_w_in = din("dn_w_in", [n_dn, D, 6208])
    dn_conv_w = din("dn_conv_w", [n_dn, 5, 4096])
    dn_a_log = din("dn_a_log", [n_dn, 32])
    dn_dt_bias = din("dn_dt_bias", [n_dn, 32])
    dn_norm_g = din("dn_norm_g", [n_dn, 128])
    dn_w_out = din("dn_w_out", [n_dn, 2048, D])
    att_w_in = din("att_w_in", [n_att, D, 1536])
    att_qn_g = din("att_qn_g", [n_att, 128])
    att_kn_g = din("att_kn_g", [n_att, 128])
    att_w_out = din("att_w_out", [n_att, D, D])
    peer_wq = din("peer_w_query", [depth, D, 2048])
    peer_sk = din("peer_sub_keys", [depth, 8, 2, 128, 128])
    peer_u = din("peer_u", [depth, 16384, D])
    peer_v = din("peer_v", [depth, 16384, D])
    c_ident = din("c_ident", [128, 128])
    c_masks = din("c_masks", [64, 6, 64])
    c_rope = din("c_rope", [T, 2, 64])
    c_iota = din("c_iota", [128, 16])
    yout = nc.dram_tensor("yout", [NB, T, D], F32, kind="ExternalOutput").ap()

    XS = dscr("XS", [NB, NT, D])
    MOD = dscr("MOD", [depth, 3, 6 * D])
    QKT = dscr("QKT", [NB, 128, 16, NT])
    VV = dscr("VV", [NB, NT, 2048])
    ZZ = dscr("ZZ", [NB, NT, 2048])
    GB = dscr("GB", [NB, NT, 64])
    OO = dscr("OO", [2, NB, NT, 2048])
    AQT = dscr("AQT", [NB, 128, 8, NT], BF16)
    AKT = dscr("AKT", [NB, 128, 2, NT], BF16)
    AVV = dscr("AVV", [NB, NT, 2, 128], BF16)
    AO = dscr("AO", [NB, NT, D])

    es0 = contextlib.ExitStack()
    with es0:
        tk = TK(nc, es0)

        uid = [0]

        def sb(es, name, shape, dt=F32):
            uid[0] += 1
            return es.enter_context(nc.sbuf_tensor("%s_u%d" % (name, uid[0]), list(shape), dt))

        def ps(es, name, shape, dt=F32):
            uid[0] += 1
            return es.enter_context(nc.psum_tensor("%s_u%d" % (name, uid[0]), list(shape), dt))

        bXS = [[Buf() for _ in range(NT // 128)] for _ in range(NB)]
        bMOD = Buf()
        bQKT, bVV, bZZ, bGB, bOO = Buf(), Buf(), Buf(), Buf(), Buf()
        bAQ, bAK, bAV, bAO = Buf(), Buf(), Buf(), Buf()
        bOUT = Buf()

        ident = sb(es0, "ident", [128, 128])
        identb = sb(es0, "identb", [128, 128], BF16)
        ones = sb(es0, "ones", [128, 128])
        masks = sb(es0, "masks", [64, 6, 64])
        iota16 = sb(es0, "iota16", [128, 16])
        condT = sb(es0, "condT", [128, 8, 3])
        bC = Buf()
        tk.dma("sp", [], [bC], ident[:], c_ident)
        if has_dn:
            tk.dma("sp", [], [bC], masks[:], c_masks)
        if cfg.peer:
            tk.dma("sp", [], [bC], iota16[:], c_iota)
        tk.op("dve", [], [bC], "memset", ones[:], 1.0)
        tk.op("dve", [bC], [bC], "tensor_copy", out=identb[:], in_=ident[:])

        def load_bc(es, name, row_ap, n=128, q="sp", buf=None):
            F = row_ap.shape[-1]
            t = sb(es, name, [n, F])
            b = buf or Buf()
            tk.dma(q, [bMOD], [b], t[:], row_ap.to_broadcast([n, F]))
            return t, b

        import os
        BIS = float(os.environ.get("BISECT", "99"))
        with contextlib.ExitStack() as es:
          if BIS > -3 and (has_dn or has_att or cfg.peer):
              crow = sb(es, "crow", [3, D])
              csil = sb(es, "csil", [3, D])
              pT = ps(es, "ada_pT", [128, 8, 4])
              pM = [ps(es, "ada_pM%d" % i, [3, 512]) for i in range(2)]
              wblk = [sb(es, "ada_w%d" % i, [128, 8, 512]) for i in range(2)]
              brow = sb(es, "ada_brow", [1, 6 * D])
              modrow = sb(es, "modrow", [3, 6 * D])
              ng = sb(es, "ada_ng", [3, 2, D])
              bc_, bw, bpm, bbr, bmr, bng, bpT = Buf(), [Buf(), Buf()], [Buf(), Buf()], Buf(), Buf(), Buf(), Buf()
              if BIS >= -0.5:
                  tk.dma("sp", [], [bc_], crow[:], cnd)
              if BIS >= 0.3:
                  tk.op("act", [bc_], [bc_], "activation", out=csil[:], in_=crow[:], func=AF.Silu)
              for k in range(8 if BIS >= 0.6 else 0):
                  tk.op("pe", [bc_, bC], [bpT], "transpose", out=pT[:, k, 0:3], in_=csil[0:3, k * 128:(k + 1) * 128],
                        identity=ident[0:3, 0:3])
              if BIS >= -0.2:
                  tk.op("dve", [bpT], [bC], "tensor_copy", out=condT[:], in_=pT[:, :, 0:3])
              for l in range(depth if BIS >= 2 else 0):
                  tk.dma("sp", [], [bbr], brow[:], ada_b[l:l + 1, :])
                  tk.dma("sp", [], [bng], ng[:, 0, :], norm1_g[l:l + 1, :].to_broadcast([3, D]))
                  tk.dma("sp", [], [bng], ng[:, 1, :], norm2_g[l:l + 1, :].to_broadcast([3, D]))
                  for cg in range(12 if BIS >= 3 else 0):
                      i = cg % 2
                      tk.dma("sp", [], [bw[i]], wblk[i][:],
                             ada_w[l, :, cg * 512:(cg + 1) * 512].rearrange("(k p) c -> p k c", p=128))
                      for k in range(8):
                          tk.op("pe", [bC, bw[i]], [bpm[i]], "matmul", pM[i][:], lhsT=condT[:, k, :], rhs=wblk[i][:, k, :],
                                start=(k == 0), stop=False)
                      tk.op("pe", [bC, bbr], [bpm[i]], "matmul", pM[i][:], lhsT=ones[0:1, 0:3],
                            rhs=brow[0:1, cg * 512:(cg + 1) * 512], start=False, stop=True)
                      tk.op("dve", [bpm[i]], [bmr], "tensor_copy", out=modrow[:, cg * 512:(cg + 1) * 512], in_=pM[i][:])
                  for j, off in ((0, 1), (1, 4)):
                      sl = modrow[:, off * D:(off + 1) * D]
                      tk.op("dve", [bmr, bng], [bmr], "scalar_tensor_tensor", out=sl, in0=sl, scalar=1.0, in1=ng[:, j, :],
                            op0=ALU.add, op1=ALU.mult)
                  if BIS >= 4:
                      tk.dma("sp", [bmr], [bMOD], MOD[l], modrow[:])
              tk.barrier()

        def seg_tiles(b):
            out = []
            for t in range(NT // 128):
                isc = t < CL // 128
                out.append((t, 2 if isc else b, isc))
            return out

        class NormCtx:
            def __init__(self, es, pfx, need_tm=False):
                self.xt = [sb(es, pfx + "_xt%d" % i, [128, D]) for i in range(2)]
                self.bxt = [Buf(), Buf()]
                self.junk = sb(es, pfx + "_junk", [128, D])
                self.st = sb(es, pfx + "_st", [128, 4])
                self.h = sb(es, pfx + "_h", [128, D])
                self.hb = sb(es, pfx + "_hb", [128, D], BF16)
                self.pT = ps(es, pfx + "_pT", [128, 8, 128], BF16)
                self.bj, self.bst, self.bh, self.bhb, self.bpT = Buf(), Buf(), Buf(), Buf(), Buf()

        def norm_mod_T(N, i, sbc, bbc, bmod, hT_out, bhT):
            x = N.xt[i]
            tk.op("dve", [N.bxt[i]], [N.bj, N.bst], "scalar_tensor_tensor", out=N.junk[:], in0=x[:], scalar=1.0, in1=x[:],
                  op0=ALU.mult, op1=ALU.mult, accum_out=N.st[:, 0:1])
            tk.op("act", [N.bst], [N.bst], "activation", out=N.st[:, 1:2], in_=N.st[:, 0:1], func=AF.Sqrt, bias=EPS,
                  scale=1.0 / D)
            tk.op("dve", [N.bst], [N.bst], "reciprocal", out=N.st[:, 2:3], in_=N.st[:, 1:2])
            tk.op("dve", [N.bxt[i], N.bst, bmod], [N.bh], "scalar_tensor_tensor", out=N.h[:], in0=x[:], scalar=N.st[:, 2:3],
                  in1=sbc[:], op0=ALU.mult, op1=ALU.mult)
            tk.op("dve", [N.bh, bmod], [N.bh], "tensor_tensor", out=N.h[:], in0=N.h[:], in1=bbc[:], op=ALU.add)
            tk.op("act", [N.bh], [N.bhb], "activation", out=N.hb[:], in_=N.h[:], func=AF.Copy)
            for k in range(8):
                tk.op("pe", [N.bhb, bC], [N.bpT], "transpose", out=N.pT[:, k, :], in_=N.hb[:, k * 128:(k + 1) * 128],
                      identity=identb[:])
            tk.op("act", [N.bpT], [bhT], "activation", out=hT_out, in_=N.pT[:], func=AF.Copy)

        def load_w_bf16(es, pfx, dst, bdst, src_ap, ncols, kch):
            with contextlib.ExitStack() as e2:
                stg = [sb(e2, pfx + "_stg%d" % i, [128, 2048]) for i in range(2)]
                bs = [Buf(), Buf()]
                n = 0
                for k in range(kch):
                    for c0 in range(0, ncols, 2048):
                        cw = min(2048, ncols - c0)
                        i = n % 2
                        n += 1
                        tk.dma("sp", [], [bs[i]], stg[i][:, 0:cw], src_ap[k * 128:(k + 1) * 128, c0:c0 + cw])
                        tk.op("dve", [bs[i]], [bdst], "tensor_copy", out=dst[:, k, c0:c0 + cw], in_=stg[i][:, 0:cw])
                tk.barrier()

        for b in range(NB if BIS != -2 else 0):
            for t in range(NT // 128):
                tk.dma("sp", [], [bXS[b][t]], XS[b, t * 128:(t + 1) * 128, :], xin[b, t * 128:(t + 1) * 128, :])

        def peer_phase(l):
            with contextlib.ExitStack() as es:
                Wq = sb(es, "pr_Wq", [128, 8, 2048], BF16)
                bWq = Buf()
                load_w_bf16(es, "pr", Wq, bWq, peer_wq[l], 2048, 8)
                skT = sb(es, "pr_skT", [128, 16, 128])
                bsk = Buf()
                N = NormCtx(es, "pr")
                hT = sb(es, "pr_hT", [128, 8, 128], BF16)
                bhT = Buf()
                pQ = ps(es, "pr_pQ", [128, 2048])
                bpQ = Buf()
                qT = sb(es, "pr_qT", [128, 16, 128])
                bqT = Buf()
                S1 = sb(es, "pr_S1", [128, 16, 128])
                S2 = sb(es, "pr_S2", [128, 16, 128])
                bS1, bS2 = Buf(), Buf()
                tops = sb(es, "pr_tops", [128, 16, 16])
                topu = sb(es, "pr_topu", [128, 16, 16], U32)
                topf = sb(es, "pr_topf", [128, 16, 16])
                btop = Buf()
                cand = sb(es, "pr_cand", [128, 8, 256])
                cand2 = sb(es, "pr_cand2", [128, 8, 256])
                bcand, bcand2 = Buf(), Buf()
                best = sb(es, "pr_best", [128, 8, 16])
                posu = sb(es, "pr_posu", [128, 8, 16], U32)
                pau = sb(es, "pr_pau", [128, 8, 16], U32)
                pbu = sb(es, "pr_pbu", [128, 8, 16], U32)
                paf = sb(es, "pr_paf", [128, 8, 16])
                pbf = sb(es, "pr_pbf", [128, 8, 16])
                bbest = Buf()
                oh = sb(es, "pr_oh", [128, 8, 16, 16])
                boh = Buf()
                isel = sb(es, "pr_isel", [128, 128])
                jsel = sb(es, "pr_jsel", [128, 128])
                idxf = sb(es, "pr_idxf", [128, 128])
                idxi = sb(es, "pr_idxi", [128, 128], I32)
                bidx = Buf()
                gate = sb(es, "pr_gate", [128, 8, 16])
                gsum = sb(es, "pr_gsum", [128, 8])
                bgate = Buf()
                actv = sb(es, "pr_actv", [128, 128])
                coef = sb(es, "pr_coef", [128, 128])
                bact, bcoef = Buf(), Buf()
                NG = 4
                gbuf = [sb(es, "pr_g%d" % i, [128, D]) for i in range(NG)]
                bg = [Buf() for _ in range(NG)]
                junk2 = sb(es, "pr_junk2", [128, D])
                bj2 = Buf()
                acc = sb(es, "pr_acc", [128, D])
                bacc = Buf()
                skl = sb(es, "pr_skl", [128, 16, 128])
                tk.dma("sp", [], [bsk], skl[:], peer_sk[l].rearrange("h c k d -> k (h c) d"))
                for g4 in range(4):
                    for j in range(4):
                        hc = g4 * 4 + j
                        tk.op("pe", [bsk, bC], [bpQ], "transpose", out=pQ[:, j * 128:(j + 1) * 128], in_=skl[:, hc, :],
                              identity=ident[:])
                    tk.op("dve", [bpQ], [bqT], "tensor_copy", out=skT[:, g4 * 4:(g4 + 1) * 4, :],
                          in_=pQ[:, 0:512].rearrange("p (a b) -> p a b", b=128))
                cur_n = None
                modt = None
                for b in range(NB):
                    for (t, n, isc) in seg_tiles(b):
                        if isc and l == depth - 1:
                            continue
                        if n != cur_n:
                            cur_n = n
                            if modt is None:
                                bm = Buf()
                                modt = [sb(es, "pr_mod%d" % j, [128, D]) for j in range(3)]
                            for j, w in enumerate((4, 3, 5)):
                                tk.dma("sp", [bMOD], [bm], modt[j][:], MOD[l, n:n + 1, w * D:(w + 1) * D].to_broadcast([128, D]))
                        i = 0
                        tk.dma("sp", [bXS[b][t]], [N.bxt[i]], N.xt[i][:], XS[b, t * 128:(t + 1) * 128, :])
                        norm_mod_T(N, i, modt[0], modt[1], bm, hT[:], bhT)
                        for g4 in range(4):
                            for j in range(4):
                                ch = g4 * 4 + j
                                for k in range(8):
                                    tk.op("pe", [bhT, bWq], [bpQ], "matmul", pQ[:, j * 128:(j + 1) * 128],
                                          lhsT=Wq[:, k, ch * 128:(ch + 1) * 128], rhs=hT[:, k, :], start=(k == 0), stop=(k == 7))
                            tk.op("act", [bpQ], [bqT], "activation", out=qT[:, g4 * 4:(g4 + 1) * 4, :],
                                  in_=pQ[:, 0:512].rearrange("p (a b) -> p a b", b=128), func=AF.Copy)
                        for hc in range(16):
                            tk.op("pe", [bqT], [bpQ], "matmul", pQ[:, hc * 128:(hc + 1) * 128], lhsT=qT[:, hc, :],
                                  rhs=skT[:, hc, :], start=True, stop=True)
                        tk.op("dve", [bpQ], [bS1], "tensor_copy", out=S1[:], in_=pQ[:].rearrange("p (a b) -> p a b", b=128))
                        for hc in range(16):
                            tk.op("dve", [bS1], [btop], "max", out=tops[:, hc, 0:8], in_=S1[:, hc, :])
                            tk.op("dve", [bS1, btop], [btop], "max_index", out=topu[:, hc, 0:8], in_max=tops[:, hc, 0:8],
                                  in_values=S1[:, hc, :])
                            tk.op("dve", [bS1, btop], [bS2], "match_replace", out=S2[:, hc, :], in_to_replace=tops[:, hc, 0:8],
                                  in_values=S1[:, hc, :], imm_value=-1e30)
                            tk.op("dve", [bS2], [btop], "max", out=tops[:, hc, 8:16], in_=S2[:, hc, :])
                            tk.op("dve", [bS2, btop], [btop], "max_index", out=topu[:, hc, 8:16], in_max=tops[:, hc, 8:16],
                                  in_values=S2[:, hc, :])
                        tk.op("dve", [btop], [btop], "tensor_copy", out=topf[:], in_=topu[:])
                        tb_ = tops[:]
                        in0 = bc_ap(tb_, [(32, 8), (1, 16), (0, 16)])
                        in1 = bass.AP(tb_.tensor, tb_.offset + 16, [list(tb_.ap[0]), [32, 8], [0, 16], [1, 16]])
                        tk.op("dve", [btop], [bcand], "tensor_tensor", out=cand[:].rearrange("p h (a b) -> p h a b", b=16),
                              in0=in0, in1=in1, op=ALU.add)
                        for h in range(8):
                            tk.op("dve", [bcand], [bbest], "max", out=best[:, h, 0:8], in_=cand[:, h, :])
                            tk.op("dve", [bcand, bbest], [bbest], "max_index", out=posu[:, h, 0:8], in_max=best[:, h, 0:8],
                                  in_values=cand[:, h, :])
                            tk.op("dve", [bcand, bbest], [bcand2], "match_replace", out=cand2[:, h, :],
                                  in_to_replace=best[:, h, 0:8], in_values=cand[:, h, :], imm_value=-1e30)
                            tk.op("dve", [bcand2], [bbest], "max", out=best[:, h, 8:16], in_=cand2[:, h, :])
                            tk.op("dve", [bcand2, bbest], [bbest], "max_index", out=posu[:, h, 8:16], in_max=best[:, h, 8:16],
                                  in_values=cand2[:, h, :])
                        tk.op("dve", [bbest], [bbest], "tensor_single_scalar", out=pau[:], in_=posu[:], scalar=4,
                              op=ALU.logical_shift_right)
                        tk.op("dve", [bbest], [bbest], "tensor_single_scalar", out=pbu[:], in_=posu[:], scalar=15,
                              op=ALU.bitwise_and)
                        tk.op("dve", [bbest], [bbest], "tensor_copy", out=paf[:], in_=pau[:])
                        tk.op("dve", [bbest], [bbest], "tensor_copy", out=pbf[:], in_=pbu[:])
                        io = iota16[:]
                        iob = bc_ap(io, [(0, 8), (0, 16), (1, 16)])
                        tf = topf[:]
                        for which, pf, dst in ((0, paf, isel), (1, pbf, jsel)):
                            pfb = bc_ap(pf[:], [(16, 8), (1, 16), (0, 16)])
                            tk.op("dve", [bbest, bC], [boh], "tensor_tensor", out=oh[:], in0=iob, in1=pfb, op=ALU.is_equal)
                            tfb = bass.AP(tf.tensor, tf.offset + 16 * which, [list(tf.ap[0]), [32, 8], [0, 16], [1, 16]])
                            tk.op("dve", [boh, btop], [boh], "tensor_tensor", out=oh[:], in0=oh[:], in1=tfb, op=ALU.mult)
                            tk.op("dve", [boh], [bidx], "tensor_reduce", out=dst[:].rearrange("p (h k) -> p h k", k=16),
                                  in_=oh[:], axis=AX.X, op=ALU.add)
                        tk.op("dve", [bidx], [bidx], "scalar_tensor_tensor", out=idxf[:], in0=isel[:], scalar=128.0, in1=jsel[:],
                              op0=ALU.mult, op1=ALU.add)
                        tk.op("dve", [bidx], [bidx], "tensor_copy", out=idxi[:], in_=idxf[:])
                        bs_ = best[:]
                        mxb = bc_ap(bs_, [(16, 8), (0, 16)])
                        tk.op("dve", [bbest], [bgate], "tensor_tensor", out=gate[:], in0=best[:], in1=mxb, op=ALU.subtract)
                        tk.op("act", [bgate], [bgate], "activation", out=gate[:], in_=gate[:], func=AF.Exp)
                        tk.op("dve", [bgate], [bgate], "tensor_reduce", out=gsum[:], in_=gate[:], axis=AX.X, op=ALU.add)
                        tk.op("dve", [bgate], [bgate], "reciprocal", out=gsum[:], in_=gsum[:])
                        tk.op("dve", [bgate], [bgate], "tensor_tensor", out=gate[:], in0=gate[:],
                              in1=bc_ap(gsum[:], [(1, 8), (0, 16)]), op=ALU.mult)
                        if os.environ.get("PEERDBG"):
                            if not isc:
                                tl = t - CL // 128
                                tk.dma("sp", [bidx], [bOUT], yout[b, tl * 128:(tl + 1) * 128, 0:128], idxf[:])
                                tk.dma("sp", [bgate], [bOUT], yout[b, tl * 128:(tl + 1) * 128, 128:256], gate[:].rearrange("p h k -> p (h k)"))
                                tk.dma("sp", [bbest], [bOUT], yout[b, tl * 128:(tl + 1) * 128, 256:384], best[:].rearrange("p h k -> p (h k)"))
                                tk.dma("sp", [btop], [bOUT], yout[b, tl * 128:(tl + 1) * 128, 384:640], topf[:].rearrange("p h k -> p (h k)"))
                                tk.dma("sp", [btop], [bOUT], yout[b, tl * 128:(tl + 1) * 128, 640:896], tops[:].rearrange("p h k -> p (h k)"))
                            continue
                        gi = 0
                        for s in range(128):
                            j = gi % NG
                            gi += 1
                            tk.dma("pool", [bidx], [bg[j]], gbuf[j][:], peer_u.rearrange("l e d -> (l e) d"),
                                   indirect=bass.IndirectOffsetOnAxis(ap=idxi[:, s:s + 1], axis=0), element_offset=l * 16384 * D)
                            tk.op("dve", [bg[j], N.bh], [bj2, bact], "scalar_tensor_tensor", out=junk2[:], in0=gbuf[j][:], scalar=1.0,
                                  in1=N.h[:], op0=ALU.mult, op1=ALU.mult, accum_out=actv[:, s:s + 1])
                        tk.op("act", [bact], [bcoef], "activation", out=coef[:], in_=actv[:], func=AF.Gelu)
                        tk.op("dve", [bcoef, bgate], [bcoef], "tensor_tensor", out=coef[:], in0=coef[:],
                              in1=gate[:].rearrange("p h k -> p (h k)"), op=ALU.mult)
                        for s in range(128):
                            j = gi % NG
                            gi += 1
                            tk.dma("pool", [bidx], [bg[j]], gbuf[j][:], peer_v.rearrange("l e d -> (l e) d"),
                                   indirect=bass.IndirectOffsetOnAxis(ap=idxi[:, s:s + 1], axis=0), element_offset=l * 16384 * D)
                            if s == 0:
                                tk.op("dve", [bg[j], bcoef], [bacc], "tensor_scalar", out=acc[:], in0=gbuf[j][:],
                                      scalar1=coef[:, 0:1], scalar2=None, op0=ALU.mult)
                            else:
                                tk.op("dve", [bg[j], bcoef, bacc], [bacc], "scalar_tensor_tensor", out=acc[:], in0=gbuf[j][:],
                                      scalar=coef[:, s:s + 1], in1=acc[:], op0=ALU.mult, op1=ALU.add)
                        tk.op("dve", [bacc, bm], [bacc], "tensor_tensor", out=acc[:], in0=acc[:], in1=modt[2][:], op=ALU.mult)
                        tk.op("dve", [bacc, N.bxt[i]], [bacc], "tensor_tensor", out=acc[:], in0=acc[:], in1=N.xt[i][:], op=ALU.add)
                        tk.dma("sp", [bacc], [bXS[b][t]], XS[b, t * 128:(t + 1) * 128, :], acc[:])
                tk.barrier()

        def dn_phase(l, di):
            last = (l == depth - 1)
            CT = CL // 128
            segs = lambda b: ((0, CL, 2), (CL, T, b))
            with contextlib.ExitStack() as es:
                SEG = max(T, CL)
                Wg = sb(es, "dn_Wg", [128, 8, 64], BF16)
                bWg = Buf()
                load_w_bf16(es, "dng", Wg, bWg, dn_w_in[di][:, 6144:6208], 64, 8)
                cwT = sb(es, "dn_cwT", [128, 32, 5])
                bcw = Buf()
                for cc in range(32):
                    tk.dma("sp", [], [bcw], cwT[:, cc, :], dn_conv_w[di][:, cc * 128:(cc + 1) * 128].rearrange("t c -> c t"),
                           allow_slow_non_contiguous=True)
                alog, bal = load_bc(es, "dn_alog", dn_a_log[di:di + 1, :])
                dtb, bdt = load_bc(es, "dn_dtb", dn_dt_bias[di:di + 1, :])
                tk.op("act", [bal], [bal], "activation", out=alog[:], in_=alog[:], func=AF.Exp)
                tk.op("dve", [bal], [bal], "tensor_scalar", out=alog[:], in0=alog[:], scalar1=-1.0, scalar2=None, op0=ALU.mult)
                hTs = sb(es, "dn_hTs", [128, 8, SEG], BF16)
                bhTs = Buf()
                N = NormCtx(es, "dn")
                modt = [sb(es, "dn_mod%d" % j, [128, D]) for j in range(2)]
                bm = Buf()
                pG = ps(es, "dn_pG", [128, 64])
                bpG = Buf()
                gsb = sb(es, "dn_gsb", [128, 64])
                gbt = sb(es, "dn_gbt", [128, 64])
                gx = sb(es, "dn_gx", [128, 4, 32])
                bgs = Buf()
                Wc = [sb(es, "dn_Wc%d" % i, [128, 8, 128]) for i in range(2)]
                bWc = [Buf(), Buf()]
                Wcb = sb(es, "dn_Wcb", [128, 8, 128], BF16)
                bWcb = Buf()
                projT = sb(es, "dn_projT", [128, SEG + 4])
                acc = sb(es, "dn_acc", [128, SEG])
                bproj, bacc = Buf(), Buf()
                pJ = [ps(es, "dn_pJ%d" % i, [128, 512]) for i in range(2)]
                bpJ = [Buf(), Buf()]
                pTv = ps(es, "dn_pTv", [128, 4, 128])
                bpTv = Buf()
                vtm = sb(es, "dn_vtm", [128, 4, 128])
                bvtm = Buf()
                for b in range(NB):
                    for (tok0, ln, n) in segs(b):
                        for j, w in enumerate((1, 0)):
                            tk.dma("sp", [bMOD], [bm], modt[j][:], MOD[l, n:n + 1, w * D:(w + 1) * D].to_broadcast([128, D]))
                        for tt in range(ln // 128):
                            t = tok0 // 128 + tt
                            tk.dma("sp", [bXS[b][t]], [N.bxt[0]], N.xt[0][:], XS[b, t * 128:(t + 1) * 128, :])
                            norm_mod_T(N, 0, modt[0], modt[1], bm, hTs[:, :, tt * 128:(tt + 1) * 128], bhTs)
                            for k in range(8):
                                tk.op("pe", [bhTs, bWg], [bpG], "matmul", pG[:], lhsT=hTs[:, k, tt * 128:(tt + 1) * 128], rhs=Wg[:, k, :],
                                      start=(k == 0), stop=(k == 7))
                            tk.op("dve", [bpG], [bgs], "tensor_copy", out=gsb[:], in_=pG[:])
                            g4 = gsb[:].rearrange("p (d k h) -> p d k h", d=2, k=2)
                            o4 = gbt[:].rearrange("p (d k h) -> p d k h", d=2, k=2)
                            x3 = gx[:, 0, :].rearrange("p (d h) -> p d h", d=2)
                            tk.op("act", [bgs], [bgs], "activation", out=o4[:, :, 0, :], in_=g4[:, :, 0, :], func=AF.Sigmoid)
                            tk.op("dve", [bgs, bdt], [bgs], "tensor_tensor", out=x3, in0=g4[:, :, 1, :],
                                  in1=dtb[:].rearrange("p (d h) -> p d h", d=2), op=ALU.add)
                            tk.op("dve", [bgs], [bgs], "tensor_scalar", out=gx[:, 1, :], in0=gx[:, 0, :], scalar1=-1.0, scalar2=None, op0=ALU.mult)
                            tk.op("dve", [bgs], [bgs], "tensor_tensor", out=gx[:, 1, :], in0=gx[:, 1, :], in1=gx[:, 0, :], op=ALU.max)
                            tk.op("act", [bgs], [bgs], "activation", out=gx[:, 1, :], in_=gx[:, 1, :], func=AF.Exp, scale=-1.0)
                            tk.op("act", [bgs], [bgs], "activation", out=gx[:, 1, :], in_=gx[:, 1, :], func=AF.Ln, bias=1.0)
                            tk.op("dve", [bgs], [bgs], "tensor_scalar", out=gx[:, 2, :], in0=gx[:, 0, :], scalar1=0.0, scalar2=None, op0=ALU.max)
                            tk.op("dve", [bgs], [bgs], "tensor_tensor", out=gx[:, 2, :], in0=gx[:, 2, :], in1=gx[:, 1, :], op=ALU.add)
                            tk.op("dve", [bgs, bal], [bgs], "tensor_tensor", out=o4[:, :, 1, :], in0=gx[:, 2, :].rearrange("p (d h) -> p d h", d=2),
                                  in1=alog[:].rearrange("p (d h) -> p d h", d=2), op=ALU.mult)
                            tk.dma("pool", [bgs], [bGB], GB[b, t * 128:(t + 1) * 128, :], gbt[:])
                        nblk = [(c0, min(512, ln - c0)) for c0 in range(0, ln, 512)]
                        for cc in range(48):
                            i = cc % 2
                            tk.dma("sp", [], [bWc[i]], Wc[i][:], dn_w_in[di][:, cc * 128:(cc + 1) * 128].rearrange("(k p) c -> p k c", p=128))
                            tk.op("dve", [bWc[i]], [bWcb], "tensor_copy", out=Wcb[:], in_=Wc[i][:])
                            if cc < 32:
                                tk.op("dve", [], [bproj], "memset", projT[:, 0:2], 0.0)
                                tk.op("dve", [], [bproj], "memset", projT[:, ln + 2:ln + 4], 0.0)
                            dst, bdst, off = (projT, bproj, 2) if cc < 32 else (acc, bacc, 0)
                            for bi, (c0, bw) in enumerate(nblk):
                                j = bi % 2
                                for k in range(8):
                                    tk.op("pe", [bhTs, bWcb], [bpJ[j]], "matmul", pJ[j][:, 0:bw], lhsT=Wcb[:, k, :], rhs=hTs[:, k, c0:c0 + bw],
                                          start=(k == 0), stop=(k == 7))
                                tk.op("act", [bpJ[j]], [bdst], "activation", out=dst[:, off + c0:off + c0 + bw], in_=pJ[j][:, 0:bw], func=AF.Copy)
                            if cc < 32:
                                tk.op("dve", [bproj, bcw], [bacc], "tensor_scalar", out=acc[:, 0:ln], in0=projT[:, 0:ln], scalar1=cwT[:, cc, 0:1],
                                      scalar2=None, op0=ALU.mult)
                                for tp in range(1, 5):
                                    tk.op("dve", [bproj, bcw, bacc], [bacc], "scalar_tensor_tensor", out=acc[:, 0:ln], in0=projT[:, tp:tp + ln],
                                          scalar=cwT[:, cc, tp:tp + 1], in1=acc[:, 0:ln], op0=ALU.mult, op1=ALU.add)
                            tk.op("act", [bacc], [bacc], "activation", out=acc[:, 0:ln], in_=acc[:, 0:ln], func=AF.Silu)
                            if cc < 16:
                                tk.op("dve", [bacc], [bproj], "tensor_tensor", out=projT[:, 0:ln], in0=acc[:, 0:ln], in1=acc[:, 0:ln], op=ALU.mult)
                                for bi, (c0, bw) in enumerate(nblk):
                                    j = bi % 2
                                    tk.op("pe", [bproj, bC], [bpJ[j]], "matmul", pJ[j][:, 0:bw], lhsT=ones[:], rhs=projT[:, c0:c0 + bw], start=True, stop=True)
                                    tk.op("act", [bpJ[j]], [bproj], "activation", out=projT[:, c0:c0 + bw], in_=pJ[j][:, 0:bw], func=AF.Sqrt, bias=EPS)
                                tk.op("dve", [bproj], [bproj], "reciprocal", out=projT[:, 0:ln], in_=projT[:, 0:ln])
                                tk.op("dve", [bproj, bacc], [bacc], "scalar_tensor_tensor", out=acc[:, 0:ln], in0=acc[:, 0:ln],
                                      scalar=(128 ** -0.5 if cc < 8 else 1.0), in1=projT[:, 0:ln], op0=ALU.mult, op1=ALU.mult)
                                tk.dma("pool", [bacc], [bQKT], QKT[b, :, cc, tok0:tok0 + ln], acc[:, 0:ln])
                            else:
                                dd, bdd, c16 = (VV, bVV, cc - 16) if cc < 32 else (ZZ, bZZ, cc - 32)
                                for t4 in range(0, ln // 128, 4):
                                    nt4 = min(4, ln // 128 - t4)
                                    for j in range(nt4):
                                        tk.op("pe", [bacc, bC], [bpTv], "transpose", out=pTv[:, j, :], in_=acc[:, (t4 + j) * 128:(t4 + j + 1) * 128],
                                              identity=ident[:])
                                    tk.op("dve", [bpTv], [bvtm], "tensor_copy", out=vtm[:, 0:nt4, :], in_=pTv[:, 0:nt4, :])
                                    r0 = tok0 + t4 * 128
                                    tk.dma("pool", [bvtm], [bdd], dd[b, r0:r0 + nt4 * 128, c16 * 128:(c16 + 1) * 128].rearrange("(j p) c -> p j c", p=128),
                                           vtm[:, 0:nt4, :])
                tk.barrier()
            with contextlib.ExitStack() as es:
                C = 64
                S32 = sb(es, "ds_S", [128, 16, 128])
                bS = Buf()
                qk = sb(es, "ds_qk", [128, 16, C])
                bqk = Buf()
                vsb = sb(es, "ds_v", [C, 16, 128])
                bv = Buf()
                gb = sb(es, "ds_gb", [C, 64])
                bgb = Buf()
                PA = ps(es, "ds_PA", [128, 2048])
                PB = ps(es, "ds_PB", [128, 2048])
                bPA, bPB = Buf(), Buf()
                sm = sb(es, "ds_sm", [128, 8, 16])
                bsm = Buf()
                ktm = sb(es, "ds_ktm", [C, 8, 128])
                bktm = Buf()
                dg = sb(es, "ds_dg", [C, 32, C])
                bdg = Buf()
                dec = sb(es, "ds_dec", [C, 16, C])
                bdec = Buf()
                U = [sb(es, "ds_U%d" % i, [C, 16, C]) for i in range(6)]
                bU = [Buf() for _ in range(6)]
                Lm = [sb(es, "ds_L%d" % i, [C, 16, C]) for i in range(2)]
                bL = [Buf(), Buf()]
                qkm = sb(es, "ds_qkm", [C, 16, C])
                bqkm = Buf()
                yv = [sb(es, "ds_y%d" % i, [C, 16, 128]) for i in range(2)]
                by = [Buf(), Buf()]
                tmpv = sb(es, "ds_tmpv", [C, 16, 128])
                btmp = Buf()
                osb = sb(es, "ds_o", [C, 16, 128])
                bo = Buf()

                def P3(Pt, n, w):
                    return Pt[0:C, 0:n * w].rearrange("p (h c) -> p h c", c=w)

                for b in range(NB):
                    for d in range(2):
                        mk = masks[:, 3 * d:3 * d + 3, :]
                        tk.op("dve", [], [bS], "memset", S32[:], 0.0)
                        order = []
                        for (tok0, ln, n) in segs(b):
                            cs = list(range(tok0, tok0 + ln, C))
                            order += cs if d == 0 else cs[::-1]
                        for t0 in order:
                            if last and t0 < CL:
                                pass
                            tk.dma("sp", [bQKT], [bqk], qk[:], QKT[b, :, :, t0:t0 + C])
                            tk.dma("sp", [bVV], [bv], vsb[:], VV[b, t0:t0 + C, :].rearrange("p (h c) -> p h c", c=128))
                            tk.dma("sp", [bGB], [bgb], gb[:], GB[b, t0:t0 + C, :])
                            beta = gb[:, d * 32:d * 32 + 16]
                            gg = gb[:, d * 32 + 16:d * 32 + 32]
                            tk.op("pe", [bC, bgb], [bPB], "matmul", PB[0:C, 0:16], lhsT=mk[:, 0, :], rhs=gg, start=True, stop=True)
                            tk.op("pe", [bC, bgb], [bPB], "matmul", PB[:, 16:32], lhsT=ones[0:C, :], rhs=gg, start=True, stop=True)
                            tk.op("dve", [bPB], [bsm], "tensor_copy", out=sm[0:C, 0, :], in_=PB[0:C, 0:16])
                            tk.op("act", [bPB], [bsm], "activation", out=sm[0:C, 1, :], in_=PB[0:C, 0:16], func=AF.Exp)
                            tk.op("act", [bPB], [bsm], "activation", out=sm[:, 4, :], in_=PB[:, 16:32], func=AF.Exp)
                            tk.op("dve", [bPB, bsm], [bsm], "tensor_tensor", out=sm[0:C, 5, :], in0=PB[0:C, 16:32], in1=sm[0:C, 0, :], op=ALU.subtract)
                            tk.op("act", [bsm], [bsm], "activation", out=sm[0:C, 2, :], in_=sm[0:C, 5, :], func=AF.Exp)
                            tk.op("dve", [bsm, bgb], [bsm], "tensor_tensor", out=sm[0:C, 3, :], in0=sm[0:C, 1, :], in1=beta, op=ALU.mult)
                            for hk in range(8):
                                tk.op("pe", [bqk, bC], [bPB], "transpose", out=PB[0:C, 512 + hk * 128:512 + (hk + 1) * 128], in_=qk[:, 8 + hk, :],
                                      identity=ident[:])
                            tk.op("act", [bPB], [bktm], "activation", out=ktm[:], in_=PB[0:C, 512:1536].rearrange("p (h c) -> p h c", c=128), func=AF.Copy)
                            idb = bc_ap(ident[0:C, 0:C], [(0, 16), (1, C)])
                            tk.op("dve", [bC, bsm], [bdg], "tensor_tensor", out=dg[:, 0:16, :], in0=idb, in1=bc_ap(sm[0:C, 0, :], [(1, 16), (0, C)]), op=ALU.mult)
                            tk.op("dve", [bC, bgb], [bdg], "tensor_tensor", out=dg[:, 16:32, :], in0=idb, in1=bc_ap(beta, [(1, 16), (0, C)]), op=ALU.mult)
                            for q4 in range(4):
                                tk.op("pe", [bdg, bC], [bPA], "matmul", PA[0:C, q4 * 512:(q4 + 1) * 512], lhsT=ones[0:C, 0:C],
                                      rhs=dg[:, q4 * 8:(q4 + 1) * 8, :].rearrange("p a b -> p (a b)"), start=True, stop=True)
                            gcb = P3(PA, 16, C)
                            btb = PA[0:C, 1024:2048].rearrange("p (h c) -> p h c", c=C)
                            tk.op("dve", [bPA, bsm], [bdec], "tensor_tensor", out=dec[:], in0=gcb, in1=bc_ap(sm[0:C, 0, :], [(1, 16), (0, C)]), op=ALU.subtract)
                            tk.op("dve", [bdec], [bdec], "tensor_scalar", out=dec[:], in0=dec[:], scalar1=0.0, scalar2=None, op0=ALU.min)
                            tk.op("act", [bdec], [bdec], "activation", out=dec[:], in_=dec[:], func=AF.Exp)
                            for hk in range(8):
                                tk.op("pe", [bqk], [bPB], "matmul", PB[0:C, hk * C:(hk + 1) * C], lhsT=qk[:, 8 + hk, :], rhs=qk[:, 8 + hk, :], start=True, stop=True)
                                tk.op("pe", [bqk], [bPB], "matmul", PB[0:C, 512 + hk * C:512 + (hk + 1) * C], lhsT=qk[:, 8 + hk, :], rhs=qk[:, hk, :], start=True, stop=True)
                            msk_s = bc_ap(mk[:, 2, :], [(0, 16), (1, C)])
                            msk_i = bc_ap(mk[:, 1, :], [(0, 16), (1, C)])
                            kkb = bass.AP(PB[:].tensor, PB[0:C, 0:512].offset, [list(PB[0:C, 0:512].ap[0]), [C, 8], [0, 2], [1, C]])
                            ptb = bass.AP(PB[:].tensor, PB[0:C, 512:1024].offset, [list(PB[0:C, 512:1024].ap[0]), [C, 8], [0, 2], [1, C]])
                            U0 = U[0]
                            tk.op("dve", [bdec, bPA], [bU[0]], "tensor_tensor", out=U0[:], in0=dec[:], in1=btb, op=ALU.mult)
                            tk.op("dve", [bU[0], bC], [bU[0]], "tensor_tensor", out=U0[:], in0=U0[:], in1=msk_s, op=ALU.mult)
                            tk.op("dve", [bU[0], bPB], [bU[0]], "tensor_tensor", out=U0[:].rearrange("p (a r) c -> p a r c", r=2),
                                  in0=U0[:].rearrange("p (a r) c -> p a r c", r=2), in1=kkb, op=ALU.mult)
                            tk.op("dve", [bdec, bC], [bqkm], "tensor_tensor", out=qkm[:], in0=dec[:], in1=msk_i, op=ALU.mult)
                            tk.op("dve", [bqkm, bPB], [bqkm], "tensor_tensor", out=qkm[:].rearrange("p (a r) c -> p a r c", r=2),
                                  in0=qkm[:].rearrange("p (a r) c -> p a r c", r=2), in1=ptb, op=ALU.mult)
                            for h in range(16):
                                tk.op("pe", [bU[0], bC], [bPA], "transpose", out=PA[0:C, h * C:(h + 1) * C], in_=U0[:, h, :], identity=ident[0:C, 0:C])
                            tk.op("act", [bPA], [bL[0]], "activation", out=Lm[0][:], in_=P3(PA, 16, C), func=AF.Copy)
                            Pcur, bPcur, Pnxt, bPnxt = PB, bPB, PA, bPA
                            for p in range(5):
                                Lc, Uc = Lm[p % 2], U[p]
                                for h in range(16):
                                    tk.op("pe", [bL[p % 2], bU[p]], [bPcur], "matmul", Pcur[0:C, h * C:(h + 1) * C], lhsT=Uc[:, h, :], rhs=Lc[:, h, :], start=True, stop=True)
                                    tk.op("pe", [bL[p % 2], bU[p]], [bPcur], "matmul", Pcur[0:C, 1024 + h * C:1024 + (h + 1) * C], lhsT=Lc[:, h, :], rhs=Uc[:, h, :], start=True, stop=True)
                                tk.op("act", [bPcur], [bL[(p + 1) % 2]], "activation", out=Lm[(p + 1) % 2][:], in_=P3(Pcur, 16, C), func=AF.Copy)
                                tk.op("dve", [bPcur], [bU[p + 1]], "tensor_copy", out=U[p + 1][:], in_=Pcur[0:C, 1024:2048].rearrange("p (h c) -> p h c", c=C))
                                Pcur, bPcur, Pnxt, bPnxt = Pnxt, bPnxt, Pcur, bPcur
                            for h in range(16):
                                tk.op("pe", [bqk, bS], [bPcur], "matmul", Pcur[0:C, h * 128:(h + 1) * 128], lhsT=qk[:, 8 + h // 2, :], rhs=S32[:, h, :], start=True, stop=True)
                            tk.op("dve", [bPcur, bsm], [btmp], "tensor_tensor", out=tmpv[:], in0=P3(Pcur, 16, 128), in1=bc_ap(sm[0:C, 3, :], [(1, 16), (0, 128)]), op=ALU.mult)
                            tk.op("dve", [bv, bgb], [by[0]], "tensor_tensor", out=yv[0][:], in0=vsb[:], in1=bc_ap(beta, [(1, 16), (0, 128)]), op=ALU.mult)
                            tk.op("dve", [by[0], btmp], [by[0]], "tensor_tensor", out=yv[0][:], in0=yv[0][:], in1=tmpv[:], op=ALU.subtract)
                            Pcur, bPcur, Pnxt, bPnxt = Pnxt, bPnxt, Pcur, bPcur
                            yi = 0
                            for p in (5, 4, 3, 2, 1, 0):
                                for h in range(16):
                                    tk.op("pe", [bU[p], by[yi]], [bPcur], "matmul", Pcur[0:C, h * 128:(h + 1) * 128], lhsT=U[p][:, h, :], rhs=yv[yi][:, h, :], start=True, stop=True)
                                tk.op("dve", [bPcur, by[yi]], [by[1 - yi]], "tensor_tensor", out=yv[1 - yi][:], in0=yv[yi][:], in1=P3(Pcur, 16, 128),
                                      op=(ALU.add if p > 0 else ALU.subtract))
                                yi = 1 - yi
                                Pcur, bPcur, Pnxt, bPnxt = Pnxt, bPnxt, Pcur, bPcur
                            vn, bvn = yv[yi], by[yi]
                            for h in range(16):
                                tk.op("pe", [bqk, bS], [bPcur], "matmul", Pcur[0:C, h * 128:(h + 1) * 128], lhsT=qk[:, h // 2, :], rhs=S32[:, h, :], start=True, stop=True)
                            tk.op("dve", [bPcur, bsm], [bo], "tensor_tensor", out=osb[:], in0=P3(Pcur, 16, 128), in1=bc_ap(sm[0:C, 1, :], [(1, 16), (0, 128)]), op=ALU.mult)
                            Pcur, bPcur, Pnxt, bPnxt = Pnxt, bPnxt, Pcur, bPcur
                            for h in range(16):
                                tk.op("pe", [bqkm, bvn], [bPcur], "matmul", Pcur[0:C, h * 128:(h + 1) * 128], lhsT=qkm[:, h, :], rhs=vn[:, h, :], start=True, stop=True)
                            tk.op("dve", [bPcur, bo], [bo], "tensor_tensor", out=osb[:], in0=osb[:], in1=P3(Pcur, 16, 128), op=ALU.add)
                            Pcur, bPcur, Pnxt, bPnxt = Pnxt, bPnxt, Pcur, bPcur
                            tk.dma("pool", [bo], [bOO], OO[d, b, t0:t0 + C, :].rearrange("p (h c) -> p h c", c=128), osb[:])
                            tk.op("dve", [bvn, bsm], [btmp], "tensor_tensor", out=tmpv[:], in0=vn[:], in1=bc_ap(sm[0:C, 2, :], [(1, 16), (0, 128)]), op=ALU.mult)
                            for h in range(16):
                                tk.op("pe", [bktm, btmp], [bPcur], "matmul", Pcur[:, h * 128:(h + 1) * 128], lhsT=ktm[:, h // 2, :], rhs=tmpv[:, h, :], start=True, stop=True)
                            tk.op("dve", [bS, bsm], [bS], "tensor_tensor", out=S32[:], in0=S32[:], in1=bc_ap(sm[:, 4, :], [(1, 16), (0, 128)]), op=ALU.mult)
                            tk.op("dve", [bS, bPcur], [bS], "tensor_tensor", out=S32[:], in0=S32[:], in1=Pcur[:, :].rearrange("p (h c) -> p h c", c=128), op=ALU.add)
                tk.barrier()
            with contextlib.ExitStack() as es:
                Wo = sb(es, "dn_Wo", [128, 16, D], BF16)
                bWo = Buf()
                load_w_bf16(es, "dno", Wo, bWo, dn_w_out[di], D, 16)
                ng_, bng = load_bc(es, "dn_ng", dn_norm_g[di:di + 1, :])
                of = sb(es, "dc_of", [128, 2048])
                ob = sb(es, "dc_ob", [128, 2048])
                zt = sb(es, "dc_z", [128, 2048])
                bof, bob, bz = Buf(), Buf(), Buf()
                sq = sb(es, "dc_sq", [128, 2048])
                ss = sb(es, "dc_ss", [128, 16])
                bsq = Buf()
                onb = sb(es, "dc_onb", [128, 2048], BF16)
                bonb = Buf()
                xt = sb(es, "dc_x", [128, D])
                bx = Buf()
                pT = ps(es, "dc_pT", [128, 16, 128], BF16)
                bpT = Buf()
                oT = sb(es, "dc_oT", [128, 16, 128], BF16)
                boT = Buf()
                pY = ps(es, "dc_pY", [128, D])
                bpY = Buf()
                yt = sb(es, "dc_y", [128, D])
                by_ = Buf()
                g1t = sb(es, "dc_g1", [128, D])
                bg1 = Buf()
                cur_n = None
                for b in range(NB):
                    for (t, n, isc) in seg_tiles(b):
                        if isc and last:
                            continue
                        if n != cur_n:
                            cur_n = n
                            tk.dma("sp", [bMOD], [bg1], g1t[:], MOD[l, n:n + 1, 2 * D:3 * D].to_broadcast([128, D]))
                        rs = slice(t * 128, (t + 1) * 128)
                        tk.dma("sp", [bOO], [bof], of[:], OO[0, b, rs, :])
                        tk.dma("sp", [bOO], [bob], ob[:], OO[1, b, rs, :])
                        tk.dma("sp", [bZZ], [bz], zt[:], ZZ[b, rs, :])
                        tk.dma("sp", [bXS[b][t]], [bx], xt[:], XS[b, rs, :])
                        tk.op("dve", [bof, bob], [bof], "tensor_tensor", out=of[:], in0=of[:], in1=ob[:], op=ALU.add)
                        tk.op("dve", [bof], [bsq], "tensor_tensor", out=sq[:], in0=of[:], in1=of[:], op=ALU.mult)
                        tk.op("dve", [bsq], [bsq], "tensor_reduce", out=ss[:], in_=sq[:].rearrange("p (h c) -> p h c", c=128), axis=AX.X, op=ALU.add)
                        tk.op("act", [bsq], [bsq], "activation", out=ss[:], in_=ss[:], func=AF.Sqrt, bias=EPS, scale=1.0 / 128)
                        tk.op("dve", [bsq], [bsq], "reciprocal", out=ss[:], in_=ss[:])
                        o3 = of[:].rearrange("p (h c) -> p h c", c=128)
                        tk.op("dve", [bof, bsq], [bof], "tensor_tensor", out=o3, in0=o3, in1=bc_ap(ss[:], [(1, 16), (0, 128)]), op=ALU.mult)
                        tk.op("dve", [bof, bng], [bof], "tensor_tensor", out=o3, in0=o3, in1=bc_ap(ng_[:], [(0, 16), (1, 128)]), op=ALU.mult)
                        tk.op("dve", [bof, bz], [bonb], "tensor_tensor", out=onb[:], in0=of[:], in1=zt[:], op=ALU.mult)
                        for k in range(16):
                            tk.op("pe", [bonb, bC], [bpT], "transpose", out=pT[:, k, :], in_=onb[:, k * 128:(k + 1) * 128], identity=identb[:])
                        tk.op("act", [bpT], [boT], "activation", out=oT[:], in_=pT[:], func=AF.Copy)
                        for cg in range(2):
                            for k in range(16):
                                tk.op("pe", [boT, bWo], [bpY], "matmul", pY[:, cg * 512:(cg + 1) * 512], lhsT=oT[:, k, :],
                                      rhs=Wo[:, k, cg * 512:(cg + 1) * 512], start=(k == 0), stop=(k == 15))
                        tk.op("dve", [bpY, bg1], [by_], "tensor_tensor", out=yt[:], in0=pY[:], in1=g1t[:], op=ALU.mult)
                        tk.op("dve", [by_, bx], [by_], "tensor_tensor", out=yt[:], in0=yt[:], in1=xt[:], op=ALU.add)
                        tk.dma("pool", [by_], [bXS[b][t]], XS[b, rs, :], yt[:])
                tk.barrier()

        def att_phase(l, ai):
            last = (l == depth - 1)
            NTT = NT // 128
            CT = CL // 128
            with contextlib.ExitStack() as es:
                Win = sb(es, "at_Win", [128, 8, 1536], BF16)
                bWin = Buf()
                load_w_bf16(es, "at", Win, bWin, att_w_in[ai], 1536, 8)
                gqk = sb(es, "at_gqk", [128, 10, 128])
                bg_ = Buf()
                for hh in range(10):
                    src = (att_qn_g if hh < 8 else att_kn_g)[ai:ai + 1, :]
                    tk.dma("sp", [], [bg_], gqk[:, hh, :], src.to_broadcast([128, 128]))
                N = NormCtx(es, "at")
                hT = sb(es, "at_hT", [128, 8, 128], BF16)
                bhT = Buf()
                pP = ps(es, "at_pP", [128, 1536])
                bpP = Buf()
                p_sb = sb(es, "at_p", [128, 1536])
                bp = Buf()
                sq = sb(es, "at_sq", [128, 1280])
                ss = sb(es, "at_ss", [128, 10])
                bsq = Buf()
                rope = sb(es, "at_rope", [128, 2, 64])
                brope = Buf()
                ra = sb(es, "at_ra", [128, 10, 64])
                rb = sb(es, "at_rb", [128, 10, 64])
                rr = sb(es, "at_rr", [128, 1280])
                brr = Buf()
                qkb = sb(es, "at_qkb", [128, 1280], BF16)
                vb = sb(es, "at_vb", [128, 256], BF16)
                bqkb, bvb = Buf(), Buf()
                pT2 = ps(es, "at_pT2", [128, 10, 128], BF16)
                bpT2 = Buf()
                qkT = sb(es, "at_qkT", [128, 10, 128], BF16)
                bqkT = Buf()
                modt = [sb(es, "at_mod%d" % j, [128, D]) for j in range(2)]
                bm = Buf()
                cur_n = None
                for b in range(NB):
                    for (t, n, isc) in seg_tiles(b):
                        if n != cur_n:
                            cur_n = n
                            for j, w in enumerate((1, 0)):
                                tk.dma("sp", [bMOD], [bm], modt[j][:], MOD[l, n:n + 1, w * D:(w + 1) * D].to_broadcast([128, D]))
                        tk.dma("sp", [bXS[b][t]], [N.bxt[0]], N.xt[0][:], XS[b, t * 128:(t + 1) * 128, :])
                        norm_mod_T(N, 0, modt[0], modt[1], bm, hT[:], bhT)
                        for cg in range(3):
                            for k in range(8):
                                tk.op("pe", [bhT, bWin], [bpP], "matmul", pP[:, cg * 512:(cg + 1) * 512], lhsT=hT[:, k, :],
                                      rhs=Win[:, k, cg * 512:(cg + 1) * 512], start=(k == 0), stop=(k == 7))
                        tk.op("dve", [bpP], [bp], "tensor_copy", out=p_sb[:], in_=pP[:])
                        tk.op("dve", [bp], [bsq], "tensor_tensor", out=sq[:], in0=p_sb[:, 0:1280], in1=p_sb[:, 0:1280], op=ALU.mult)
                        tk.op("dve", [bsq], [bsq], "tensor_reduce", out=ss[:], in_=sq[:].rearrange("p (h d) -> p h d", d=128),
                              axis=AX.X, op=ALU.add)
                        tk.op("act", [bsq], [bsq], "activation", out=ss[:], in_=ss[:], func=AF.Sqrt, bias=EPS, scale=1.0 / 128)
                        tk.op("dve", [bsq], [bsq], "reciprocal", out=ss[:], in_=ss[:])
                        sq3 = sq[:].rearrange("p (h d) -> p h d", d=128)
                        tk.op("dve", [bp, bsq], [bsq], "tensor_tensor", out=sq3, in0=p_sb[:, 0:1280].rearrange("p (h d) -> p h d", d=128),
                              in1=bc_ap(ss[:], [(1, 10), (0, 128)]), op=ALU.mult)
                        tk.op("dve", [bsq, bg_], [bsq], "tensor_tensor", out=sq3, in0=sq3, in1=gqk[:], op=ALU.mult)
                        if not isc:
                            tl = t - CT
                            tk.dma("sp", [], [brope], rope[:], c_rope[tl * 128:(tl + 1) * 128])
                            s4 = sq[:].rearrange("p (h i two) -> p h i two", i=64, two=2)
                            r4 = rr[:].rearrange("p (h i two) -> p h i two", i=64, two=2)
                            xe, xo = s4[:, :, :, 0], s4[:, :, :, 1]
                            cb = bc_ap(rope[:, 0, :], [(0, 10), (1, 64)])
                            sbb = bc_ap(rope[:, 1, :], [(0, 10), (1, 64)])
                            tk.op("dve", [bsq, brope], [brr], "tensor_tensor", out=ra[:], in0=xe, in1=cb, op=ALU.mult)
                            tk.op("dve", [bsq, brope], [brr], "tensor_tensor", out=rb[:], in0=xo, in1=sbb, op=ALU.mult)
                            tk.op("dve", [brr], [brr], "tensor_tensor", out=r4[:, :, :, 0], in0=ra[:], in1=rb[:], op=ALU.subtract)
                            tk.op("dve", [bsq, brope, brr], [brr], "tensor_tensor", out=ra[:], in0=xe, in1=sbb, op=ALU.mult)
                            tk.op("dve", [bsq, brope, brr], [brr], "tensor_tensor", out=rb[:], in0=xo, in1=cb, op=ALU.mult)
                            tk.op("dve", [brr], [brr], "tensor_tensor", out=r4[:, :, :, 1], in0=ra[:], in1=rb[:], op=ALU.add)
                            src = rr
                            bsrc = brr
                        else:
                            src = sq
                            bsrc = bsq
                        tk.op("act", [bsrc], [bqkb], "activation", out=qkb[:, 0:1024], in_=src[:, 0:1024], func=AF.Copy, scale=128 ** -0.5)
                        tk.op("act", [bsrc], [bqkb], "activation", out=qkb[:, 1024:1280], in_=src[:, 1024:1280], func=AF.Copy)
                        tk.op("act", [bp], [bvb], "activation", out=vb[:], in_=p_sb[:, 1280:1536], func=AF.Copy)
                        for hh in range(10):
                            tk.op("pe", [bqkb, bC], [bpT2], "transpose", out=pT2[:, hh, :], in_=qkb[:, hh * 128:(hh + 1) * 128], identity=identb[:])
                        tk.op("dve", [bpT2], [bqkT], "tensor_copy", out=qkT[:], in_=pT2[:])
                        tk.dma("pool", [bqkT], [bAQ], AQT[b, :, :, t * 128:(t + 1) * 128], qkT[:, 0:8, :])
                        tk.dma("pool", [bqkT], [bAK], AKT[b, :, :, t * 128:(t + 1) * 128], qkT[:, 8:10, :])
                        tk.dma("pool", [bvb], [bAV], AVV[b, t * 128:(t + 1) * 128], vb[:].rearrange("p (g d) -> p g d", d=128))
                tk.barrier()
            with contextlib.ExitStack() as es:
                gq, bgq = load_bc(es, "at_gq", att_qn_g[ai:ai + 1, :])
                gk, bgk = load_bc(es, "at_gk", att_kn_g[ai:ai + 1, :])
                mm_ = sb(es, "at_mm", [128, 4])
                bmm = Buf()
                tk.op("dve", [bgq], [bmm], "tensor_reduce", out=mm_[:, 0:1], in_=gq[:], axis=AX.X, op=ALU.max, apply_absolute_value=True)
                tk.op("dve", [bgk], [bmm], "tensor_reduce", out=mm_[:, 1:2], in_=gk[:], axis=AX.X, op=ALU.max, apply_absolute_value=True)
                tk.op("dve", [bmm], [bmm], "scalar_tensor_tensor", out=mm_[:, 2:3], in0=mm_[:, 0:1], scalar=-(128 ** 0.5), in1=mm_[:, 1:2],
                      op0=ALU.mult, op1=ALU.mult)
                KT = sb(es, "at_KT", [128, NT], BF16)
                Va = sb(es, "at_Va", [128, NTT, 129], BF16)
                bKT, bVa = Buf(), Buf()
                QB = min(512, T)
                QTt = [sb(es, "at_QT%d" % i, [128, 512], BF16) for i in range(2)]
                bQT = [Buf(), Buf()]
                pS = [ps(es, "at_pS%d" % i, [128, 512]) for i in range(2)]
                bpS = [Buf(), Buf()]
                Pt = [sb(es, "at_P%d" % i, [128, 512], BF16) for i in range(2)]
                bP = [Buf(), Buf()]
                pO = [ps(es, "at_pO%d" % i, [128, 512]) for i in range(4)]
                bpO = [Buf() for _ in range(4)]
                osb = [sb(es, "at_o%d" % i, [128, 128]) for i in range(2)]
                rd = [sb(es, "at_rd%d" % i, [128, 1]) for i in range(2)]
                bo = [Buf(), Buf()]
                nq = 0
                no = 0
                for b in range(NB):
                    for g in range(2):
                        tk.dma("sp", [bAK], [bKT], KT[:], AKT[b, :, g, :])
                        tk.op("dve", [], [bVa], "memset", Va[:, :, 128:129], 1.0)
                        tk.dma("sp", [bAV], [bVa], Va[:, :, 0:128], AVV[b, :, g, :].rearrange("(c p) d -> p c d", p=128))
                        blocks = [(CL + qb * QB, QB, NTT) for qb in range(T // QB)]
                        if not last:
                            blocks.append((0, CL, CT))
                        for h in range(4):
                            for (q0, qn, nkc) in blocks:
                                i = nq % 2
                                nq += 1
                                tk.dma("sp", [bAQ], [bQT[i]], QTt[i][:, 0:qn], AQT[b, :, g * 4 + h, q0:q0 + qn])
                                nqs = qn // 128
                                for kc in range(nkc):
                                    j = kc % 2
                                    tk.op("pe", [bKT, bQT[i]], [bpS[j]], "matmul", pS[j][:, 0:qn], lhsT=KT[:, kc * 128:(kc + 1) * 128],
                                          rhs=QTt[i][:, 0:qn], start=True, stop=True)
                                    tk.op("act", [bpS[j], bmm], [bP[j]], "activation", out=Pt[j][:, 0:qn], in_=pS[j][:, 0:qn], func=AF.Exp,
                                          bias=mm_[:, 2:3])
                                    for qs in range(nqs):
                                        tk.op("pe", [bP[j], bVa], [bpO[qs]], "matmul", pO[qs][:, 0:129], lhsT=Pt[j][:, qs * 128:(qs + 1) * 128],
                                              rhs=Va[:, kc, :], start=(kc == 0), stop=(kc == nkc - 1))
                                for qs in range(nqs):
                                    o_ = no % 2
                                    no += 1
                                    tk.op("dve", [bpO[qs]], [bo[o_]], "reciprocal", out=rd[o_][:], in_=pO[qs][:, 128:129])
                                    tk.op("dve", [bpO[qs], bo[o_]], [bo[o_]], "tensor_scalar", out=osb[o_][:], in0=pO[qs][:, 0:128],
                                          scalar1=rd[o_][:, 0:1], scalar2=None, op0=ALU.mult)
                                    r0 = q0 + qs * 128
                                    tk.dma("pool", [bo[o_]], [bAO], AO[b, r0:r0 + 128, (g * 4 + h) * 128:(g * 4 + h + 1) * 128], osb[o_][:])
                tk.barrier()
            with contextlib.ExitStack() as es:
                Wo = sb(es, "at_Wo", [128, 8, D], BF16)
                bWo = Buf()
                load_w_bf16(es, "ato", Wo, bWo, att_w_out[ai], D, 8)
                ao = sb(es, "at_ao", [128, D])
                aob = sb(es, "at_aob", [128, D], BF16)
                bao, baob = Buf(), Buf()
                xt = sb(es, "at_x", [128, D])
                bx = Buf()
                pT = ps(es, "at_pTc", [128, 8, 128], BF16)
                bpT = Buf()
                oT = sb(es, "at_oT", [128, 8, 128], BF16)
                boT = Buf()
                pY = ps(es, "at_pY", [128, D])
                bpY = Buf()
                yt = sb(es, "at_y", [128, D])
                by = Buf()
                g1t = sb(es, "at_g1", [128, D])
                bg1 = Buf()
                cur_n = None
                for b in range(NB):
                    for (t, n, isc) in seg_tiles(b):
                        if isc and last:
                            continue
                        if n != cur_n:
                            cur_n = n
                            tk.dma("sp", [bMOD], [bg1], g1t[:], MOD[l, n:n + 1, 2 * D:3 * D].to_broadcast([128, D]))
                        tk.dma("sp", [bAO], [bao], ao[:], AO[b, t * 128:(t + 1) * 128, :])
                        tk.dma("sp", [bXS[b][t]], [bx], xt[:], XS[b, t * 128:(t + 1) * 128, :])
                        tk.op("act", [bao], [baob], "activation", out=aob[:], in_=ao[:], func=AF.Copy)
                        for k in range(8):
                            tk.op("pe", [baob, bC], [bpT], "transpose", out=pT[:, k, :], in_=aob[:, k * 128:(k + 1) * 128], identity=identb[:])
                        tk.op("dve", [bpT], [boT], "tensor_copy", out=oT[:], in_=pT[:])
                        for cg in range(2):
                            for k in range(8):
                                tk.op("pe", [boT, bWo], [bpY], "matmul", pY[:, cg * 512:(cg + 1) * 512], lhsT=oT[:, k, :],
                                      rhs=Wo[:, k, cg * 512:(cg + 1) * 512], start=(k == 0), stop=(k == 7))
                        tk.op("dve", [bpY, bg1], [by], "tensor_tensor", out=yt[:], in0=pY[:], in1=g1t[:], op=ALU.mult)
                        tk.op("dve", [by, bx], [by], "tensor_tensor", out=yt[:], in0=yt[:], in1=xt[:], op=ALU.add)
                        tk.dma("pool", [by], [bXS[b][t]], XS[b, t * 128:(t + 1) * 128, :], yt[:])
                tk.barrier()

        def final_phase():
            with contextlib.ExitStack() as es:
                xt = [sb(es, "fn_x%d" % i, [128, D]) for i in range(2)]
                bx = [Buf(), Buf()]
                junk = sb(es, "fn_junk", [128, D])
                st = sb(es, "fn_st", [128, 4])
                bj, bst = Buf(), Buf()
                gb_, bgb = load_bc(es, "fn_g", final_g[0:1, :])
                yt = [sb(es, "fn_y%d" % i, [128, D]) for i in range(2)]
                by = [Buf(), Buf()]
                n = 0
                for b in range(NB):
                    for t in range(CL // 128, NT // 128):
                        i = n % 2
                        n += 1
                        tk.dma("sp", [bXS[b][t]], [bx[i]], xt[i][:], (XS if BIS != -2 else xin)[b, t * 128:(t + 1) * 128, :])
                        tk.op("dve", [bx[i]], [bj, bst], "scalar_tensor_tensor", out=junk[:], in0=xt[i][:], scalar=1.0, in1=xt[i][:],
                              op0=ALU.mult, op1=ALU.mult, accum_out=st[:, 0:1])
                        tk.op("act", [bst], [bst], "activation", out=st[:, 1:2], in_=st[:, 0:1], func=AF.Sqrt, bias=EPS,
                              scale=1.0 / D)
                        tk.op("dve", [bst], [bst], "reciprocal", out=st[:, 2:3], in_=st[:, 1:2])
                        tk.op("dve", [bx[i], bst, bgb], [by[i]], "scalar_tensor_tensor", out=yt[i][:], in0=xt[i][:],
                              scalar=st[:, 2:3], in1=gb_[:], op0=ALU.mult, op1=ALU.mult)
                        tl = t - CL // 128
                        tk.dma("pool", [by[i]], [bOUT], yout[b, tl * 128:(tl + 1) * 128, :], yt[i][:])
                tk.barrier()

        dn_i = att_i = 0
        for l, m in enumerate(cfg.layers):
            if m == "dn":
                dn_phase(l, dn_i)
                dn_i += 1
            elif m == "att":
                att_phase(l, att_i)
                att_i += 1
            if cfg.peer:
                peer_phase(l)
        if not os.environ.get("PEERDBG"):
            final_phase()
        tk.barrier()
        print("ninst", tk.ninst, flush=True)
    nc._in_names = in_names
    return nc


def rope_tables(T):
    rows = T // 64
    row = np.broadcast_to(np.arange(rows)[:, None], (rows, 64)).reshape(-1).astype(np.float32)
    col = np.broadcast_to(np.arange(64)[None, :], (rows, 64)).reshape(-1).astype(np.float32)
    freqs = (np.float32(10000.0) ** (-np.arange(0, 64, 2, dtype=np.float32) / np.float32(64))).astype(np.float32)
    ang = np.concatenate([row[:, None] * freqs, col[:, None] * freqs], axis=-1).astype(np.float32)
    return np.stack([np.cos(ang), np.sin(ang)], axis=1).astype(np.float32)


def const_inputs(T):
    i = np.arange(64)
    cum_f = (i[:, None] <= i[None, :]).astype(np.float32)
    incl_f = cum_f.copy()
    strict_f = (i[:, None] < i[None, :]).astype(np.float32)
    cum_b = cum_f.T.copy()
    incl_b = incl_f.T.copy()
    strict_b = strict_f.T.copy()
    masks = np.stack([cum_f, incl_f, strict_f, cum_b, incl_b, strict_b], axis=1).astype(np.float32)
    return {
        "c_ident": np.eye(128, dtype=np.float32),
        "c_masks": masks,
        "c_rope": rope_tables(T),
        "c_iota": np.broadcast_to(np.arange(16, dtype=np.float32)[None, :], (128, 16)).copy(),
    }


def make_in_maps(cfg, inputs, ncores, names=None):
    NB = cfg.NB
    f = lambda a: np.ascontiguousarray(np.asarray(a, dtype=np.float32))
    consts = const_inputs(cfg.T)
    shared = {k: f(inputs[k]) for k in ("ada_w", "ada_b", "norm1_g", "norm2_g", "dn_w_in", "dn_conv_w", "dn_norm_g",
                                         "dn_w_out", "att_w_in", "att_qn_g", "att_kn_g", "att_w_out", "peer_w_query",
                                         "peer_sub_keys", "peer_u", "peer_v")}
    shared["final_g"] = f(inputs["final_g"]).reshape(1, D)
    shared["dn_a_log"] = f(inputs["dn_a_log"]).reshape(-1, 32)
    shared["dn_dt_bias"] = f(inputs["dn_dt_bias"]).reshape(-1, 32)
    shared.update(consts)
    maps = []
    for c in range(ncores):
        bs = slice(c * NB, (c + 1) * NB)
        m = dict(shared)
        m["xin"] = np.ascontiguousarray(np.concatenate([f(inputs["ctx"])[bs], f(inputs["x"])[bs]], axis=1))
        cnd = np.zeros((3, D), np.float32)
        cnd[0:NB] = f(inputs["c"])[bs]
        cnd[2] = f(inputs["c_ctx"])
        m["cnd"] = cnd
        if names is not None:
            m = {k: v for k, v in m.items() if k in names}
        maps.append(m)
    return maps


def kernel(**inputs):
    cfg = Cfg()
    nc = build_program(cfg)
    maps = make_in_maps(cfg, inputs, NCORES, nc._in_names)
    res = run_bass_kernel_spmd(nc, maps, core_ids=list(range(NCORES)))
    return np.concatenate([r["yout"] for r in res.results], axis=0).astype(np.float32)
```

```python
import contextlib
import numpy as np
import concourse.bass as bass
import concourse.mybir as mybir
from concourse.bass_utils import run_bass_kernel_spmd

F32 = mybir.dt.float32
BF16 = mybir.dt.bfloat16
I32 = mybir.dt.int32
U32 = mybir.dt.uint32
AF = mybir.ActivationFunctionType
ALU = mybir.AluOpType
AX = mybir.AxisListType

D = 1024
EPS = 1e-6
NCORES = 8


class Buf:
    __slots__ = ("w", "r", "name")

    def __init__(self, name=""):
        self.w = None
        self.r = {}
        self.name = name


class TK:
    def __init__(self, nc, es, n_dma_sems=40):
        self.nc = nc
        self.engs = {"pe": nc.tensor, "act": nc.scalar, "dve": nc.vector, "pool": nc.gpsimd, "sp": nc.sync}
        self.sem = {k: es.enter_context(nc.semaphore("sem_" + k)) for k in self.engs}
        self.cnt = {k: 0 for k in self.engs}
        self.seen = {k: {} for k in self.engs}
        self.dsems = [es.enter_context(nc.semaphore("dsem%d" % i)) for i in range(n_dma_sems)]
        self.dissued = [0] * n_dma_sems
        self.drr = 0
        self.ninst = 0
        self.outst = {k: [] for k in self.engs}
        self.max_out = 6

    def _wait(self, eng, key, val):
        if key == ("E", "pe") and eng == "pe":
            return
        if self.seen[eng].get(key, 0) >= val:
            return
        if key[0] == "D":
            val = max(val, self.dissued[key[1]])
            sem = self.dsems[key[1]]
        else:
            sem = self.sem[key[1]]
        self.engs[eng].wait_ge(sem, val)
        self.seen[eng][key] = val
        self.ninst += 1

    def _deps(self, eng, r, w):
        for b in r:
            if b.w is not None:
                self._wait(eng, b.w[0], b.w[1])
        for b in w:
            if b.w is not None:
                self._wait(eng, b.w[0], b.w[1])
            for k, v in b.r.items():
                self._wait(eng, k, v)

    def _commit(self, key, val, r, w):
        for b in r:
            if b.r.get(key, 0) < val:
                b.r[key] = val
        for b in w:
            b.w = (key, val)
            b.r = {}

    def op(self, eng, r, w, meth, *a, **kw):
        self._deps(eng, r, w)
        inst = getattr(self.engs[eng], meth)(*a, **kw)
        inst.then_inc(self.sem[eng], 1)
        self.cnt[eng] += 1
        self.ninst += 1
        self._commit(("E", eng), self.cnt[eng], r, w)
        return inst

    def dma(self, q, r, w, out, in_, indirect=None, **kw):
        self._deps(q, r, w)
        fifo = self.outst[q]
        while len(fifo) >= self.max_out:
            k0, v0 = fifo.pop(0)
            self._wait(q, k0, v0)
        i = self.drr
        self.drr = (self.drr + 1) % len(self.dsems)
        if indirect is None:
            inst = self.engs[q].dma_start(out=out, in_=in_, **kw)
        else:
            inst = self.engs[q].indirect_dma_start(out=out, out_offset=None, in_=in_, in_offset=indirect, **kw)
        inst.then_inc(self.dsems[i], 16)
        self.dissued[i] += 16
        self.ninst += 1
        self._commit(("D", i), self.dissued[i], r, w)
        fifo.append((("D", i), self.dissued[i]))
        return inst

    def barrier(self):
        for e in self.engs:
            for o in self.engs:
                if o != e and self.cnt[o] > 0:
                    self._wait(e, ("E", o), self.cnt[o])
            for i, v in enumerate(self.dissued):
                if v > 0:
                    self._wait(e, ("D", i), v)


def bc_ap(base, dims):
    a = base.ap
    return bass.AP(base.tensor, base.offset, [list(a[0])] + [list(d) for d in dims])


class Cfg:
    def __init__(self, NB=2, T=4096, CL=256, layers=("dn", "att", "dn", "att"), peer=True):
        self.NB, self.T, self.CL, self.layers, self.peer = NB, T, CL, tuple(layers), peer
        self.NT = T + CL


def build_program(cfg):
    NB, T, CL, NT = cfg.NB, cfg.T, cfg.CL, cfg.NT
    depth = len(cfg.layers)
    n_dn = max(1, sum(1 for m in cfg.layers if m == "dn"))
    n_att = max(1, sum(1 for m in cfg.layers if m == "att"))
    nc = bass.Bass("TRN2", target_bir_lowering=False)

    in_names = []
    has_dn = "dn" in cfg.layers
    has_att = "att" in cfg.layers

    def din(name, shape, dt=F32):
        if (name.startswith("dn_") or name == "c_masks") and not has_dn:
            return None
        if (name.startswith("att_") or name == "c_rope") and not has_att:
            return None
        if (name.startswith("peer_") or name == "c_iota") and not cfg.peer:
            return None
        if name in ("cnd", "ada_w", "ada_b", "norm1_g", "norm2_g") and not (has_dn or has_att or cfg.peer):
            return None
        in_names.append(name)
        return nc.dram_tensor(name, list(shape), dt, kind="ExternalInput").ap()

    def dscr(name, shape, dt=F32):
        if name == "MOD" and not (has_dn or has_att or cfg.peer):
            return None
        if name in ("QKT", "VV", "ZZ", "GB", "OO") and not has_dn:
            return None
        if name in ("AQT", "AKT", "AVV", "AO") and not has_att:
            return None
        return nc.dram_tensor(name, list(shape), dt, kind="Internal").ap()

    xin = din("xin", [NB, NT, D])
    cnd = din("cnd", [3, D])
    ada_w = din("ada_w", [depth, D, 6 * D])
    ada_b = din("ada_b", [depth, 6 * D])
    norm1_g = din("norm1_g", [depth, D])
    norm2_g = din("norm2_g", [depth, D])
    final_g = din("final_g", [1, D])
    dn_w_in = din("dn_w_in", [n_dn, D, 6208])
    dn_conv_w = din("dn_conv_w", [n_dn, 5, 4096])
    dn_a_log = din("dn_a_log", [n_dn, 32])
    dn_dt_bias = din("dn_dt_bias", [n_dn, 32])
    dn_norm_g = din("dn_norm_g", [n_dn, 128])
    dn_w_out = din("dn_w_out", [n_dn, 2048, D])
    att_w_in = din("att_w_in", [n_att, D, 1536])
    att_qn_g = din("att_qn_g", [n_att, 128])
    att_kn_g = din("att_kn_g", [n_att, 128])
    att_w_out = din("att_w_out", [n_att, D, D])
    peer_wq = din("peer_w_query", [depth, D, 2048])
    peer_sk = din("peer_sub_keys", [depth, 8, 2, 128, 128])
    peer_u = din("peer_u", [depth, 16384, D])
    peer_v = din("peer_v", [depth, 16384, D])
    c_ident = din("c_ident", [128, 128])
    c_masks = din("c_masks", [64, 6, 64])
    c_rope = din("c_rope", [T, 2, 64])
    c_iota = din("c_iota", [128, 16])
    yout = nc.dram_tensor("yout", [NB, T, D], F32, kind="ExternalOutput").ap()

    XS = dscr("XS", [NB, NT, D])
    MOD = dscr("MOD", [depth, 3, 6 * D])
    QKT = dscr("QKT", [NB, 128, 16, NT])
    VV = dscr("VV", [NB, NT, 2048])
    ZZ = dscr("ZZ", [NB, NT, 2048])
    GB = dscr("GB", [NB, NT, 64])
    OO = dscr("OO", [2, NB, NT, 2048])
    AQT = dscr("AQT", [NB, 128, 8, NT], BF16)
    AKT = dscr("AKT", [NB, 128, 2, NT], BF16)
    AVV = dscr("AVV", [NB, NT, 2, 128], BF16)
    AO = dscr("AO", [NB, NT, D])

    es0 = contextlib.ExitStack()
    with es0:
        tk = TK(nc, es0)

        uid = [0]

        def sb(es, name, shape, dt=F32):
            uid[0] += 1
            return es.enter_context(nc.sbuf_tensor("%s_u%d" % (name, uid[0]), list(shape), dt))

        def ps(es, name, shape, dt=F32):
            uid[0] += 1
            return es.enter_context(nc.psum_tensor("%s_u%d" % (name, uid[0]), list(shape), dt))

        bXS = [[Buf() for _ in range(NT // 128)] for _ in range(NB)]
        bMOD = Buf()
        bQKT, bVV, bZZ, bGB, bOO = Buf(), Buf(), Buf(), Buf(), Buf()
        bAQ, bAK, bAV, bAO = Buf(), Buf(), Buf(), Buf()
        bOUT = Buf()

        ident = sb(es0, "ident", [128, 128])
        identb = sb(es0, "identb", [128, 128], BF16)
        ones = sb(es0, "ones", [128, 128])
        masks = sb(es0, "masks", [64, 6, 64])
        iota16 = sb(es0, "iota16", [128, 16])
        condT = sb(es0, "condT", [128, 8, 3])
        bC = Buf()
        tk.dma("sp", [], [bC], ident[:], c_ident)
        if has_dn:
            tk.dma("sp", [], [bC], masks[:], c_masks)
        if cfg.peer:
            tk.dma("sp", [], [bC], iota16[:], c_iota)
        tk.op("dve", [], [bC], "memset", ones[:], 1.0)
        tk.op("dve", [bC], [bC], "tensor_copy", out=identb[:], in_=ident[:])

        def load_bc(es, name, row_ap, n=128, q="sp", buf=None):
            F = row_ap.shape[-1]
            t = sb(es, name, [n, F])
            b = buf or Buf()
            tk.dma(q, [bMOD], [b], t[:], row_ap.to_broadcast([n, F]))
            return t, b

        import os
        BIS = float(os.environ.get("BISECT", "99"))
        with contextlib.ExitStack() as es:
          if BIS > -3 and (has_dn or has_att or cfg.peer):
              crow = sb(es, "crow", [3, D])
              csil = sb(es, "csil", [3, D])
              pT = ps(es, "ada_pT", [128, 8, 4])
              pM = [ps(es, "ada_pM%d" % i, [3, 512]) for i in range(2)]
              wblk = [sb(es, "ada_w%d" % i, [128, 8, 512]) for i in range(2)]
              brow = sb(es, "ada_brow", [1, 6 * D])
              modrow = sb(es, "modrow", [3, 6 * D])
              ng = sb(es, "ada_ng", [3, 2, D])
              bc_, bw, bpm, bbr, bmr, bng, bpT = Buf(), [Buf(), Buf()], [Buf(), Buf()], Buf(), Buf(), Buf(), Buf()
              if BIS >= -0.5:
                  tk.dma("sp", [], [bc_], crow[:], cnd)
              if BIS >= 0.3:
                  tk.op("act", [bc_], [bc_], "activation", out=csil[:], in_=crow[:], func=AF.Silu)
              for k in range(8 if BIS >= 0.6 else 0):
                  tk.op("pe", [bc_, bC], [bpT], "transpose", out=pT[:, k, 0:3], in_=csil[0:3, k * 128:(k + 1) * 128],
                        identity=ident[0:3, 0:3])
              if BIS >= -0.2:
                  tk.op("dve", [bpT], [bC], "tensor_copy", out=condT[:], in_=pT[:, :, 0:3])
              for l in range(depth if BIS >= 2 else 0):
                  tk.dma("sp", [], [bbr], brow[:], ada_b[l:l + 1, :])
                  tk.dma("sp", [], [bng], ng[:, 0, :], norm1_g[l:l + 1, :].to_broadcast([3, D]))
                  tk.dma("sp", [], [bng], ng[:, 1, :], norm2_g[l:l + 1, :].to_broadcast([3, D]))
                  for cg in range(12 if BIS >= 3 else 0):
                      i = cg % 2
                      tk.dma("sp", [], [bw[i]], wblk[i][:],
                             ada_w[l, :, cg * 512:(cg + 1) * 512].rearrange("(k p) c -> p k c", p=128))
                      for k in range(8):
                          tk.op("pe", [bC, bw[i]], [bpm[i]], "matmul", pM[i][:], lhsT=condT[:, k, :], rhs=wblk[i][:, k, :],
                                start=(k == 0), stop=False)
                      tk.op("pe", [bC, bbr], [bpm[i]], "matmul", pM[i][:], lhsT=ones[0:1, 0:3],
                            rhs=brow[0:1, cg * 512:(cg + 1) * 512], start=False, stop=True)
                      tk.op("dve", [bpm[i]], [bmr], "tensor_copy", out=modrow[:, cg * 512:(cg + 1) * 512], in_=pM[i][:])
                  for j, off in ((0, 1), (1, 4)):
                      sl = modrow[:, off * D:(off + 1) * D]
                      tk.op("dve", [bmr, bng], [bmr], "scalar_tensor_tensor", out=sl, in0=sl, scalar=1.0, in1=ng[:, j, :],
                            op0=ALU.add, op1=ALU.mult)
                  if BIS >= 4:
                      tk.dma("sp", [bmr], [bMOD], MOD[l], modrow[:])
              tk.barrier()

        def seg_tiles(b):
            out = []
            for t in range(NT // 128):
                isc = t < CL // 128
                out.append((t, 2 if isc else b, isc))
            return out

        class NormCtx:
            def __init__(self, es, pfx, need_tm=False):
                self.xt = [sb(es, pfx + "_xt%d" % i, [128, D]) for i in range(2)]
                self.bxt = [Buf(), Buf()]
                self.junk = sb(es, pfx + "_junk", [128, D])
                self.st = sb(es, pfx + "_st", [128, 4])
                self.h = sb(es, pfx + "_h", [128, D])
                self.hb = sb(es, pfx + "_hb", [128, D], BF16)
                self.pT = ps(es, pfx + "_pT", [128, 8, 128], BF16)
                self.bj, self.bst, self.bh, self.bhb, self.bpT = Buf(), Buf(), Buf(), Buf(), Buf()

        def norm_mod_T(N, i, sbc, bbc, bmod, hT_out, bhT):
            x = N.xt[i]
            tk.op("dve", [N.bxt[i]], [N.bj, N.bst], "scalar_tensor_tensor", out=N.junk[:], in0=x[:], scalar=1.0, in1=x[:],
                  op0=ALU.mult, op1=ALU.mult, accum_out=N.st[:, 0:1])
            tk.op("act", [N.bst], [N.bst], "activation", out=N.st[:, 1:2], in_=N.st[:, 0:1], func=AF.Sqrt, bias=EPS,
                  scale=1.0 / D)
            tk.op("dve", [N.bst], [N.bst], "reciprocal", out=N.st[:, 2:3], in_=N.st[:, 1:2])
            tk.op("dve", [N.bxt[i], N.bst, bmod], [N.bh], "scalar_tensor_tensor", out=N.h[:], in0=x[:], scalar=N.st[:, 2:3],
                  in1=sbc[:], op0=ALU.mult, op1=ALU.mult)
            tk.op("dve", [N.bh, bmod], [N.bh], "tensor_tensor", out=N.h[:], in0=N.h[:], in1=bbc[:], op=ALU.add)
            tk.op("act", [N.bh], [N.bhb], "activation", out=N.hb[:], in_=N.h[:], func=AF.Copy)
            for k in range(8):
                tk.op("pe", [N.bhb, bC], [N.bpT], "transpose", out=N.pT[:, k, :], in_=N.hb[:, k * 128:(k + 1) * 128],
                      identity=identb[:])
            tk.op("act", [N.bpT], [bhT], "activation", out=hT_out, in_=N.pT[:], func=AF.Copy)

        def load_w_bf16(es, pfx, dst, bdst, src_ap, ncols, kch):
            with contextlib.ExitStack() as e2:
                stg = [sb(e2, pfx + "_stg%d" % i, [128, 2048]) for i in range(2)]
                bs = [Buf(), Buf()]
                n = 0
                for k in range(kch):
                    for c0 in range(0, ncols, 2048):
                        cw = min(2048, ncols - c0)
                        i = n % 2
                        n += 1
                        tk.dma("sp", [], [bs[i]], stg[i][:, 0:cw], src_ap[k * 128:(k + 1) * 128, c0:c0 + cw])
                        tk.op("dve", [bs[i]], [bdst], "tensor_copy", out=dst[:, k, c0:c0 + cw], in_=stg[i][:, 0:cw])
                tk.barrier()

        for b in range(NB if BIS != -2 else 0):
            for t in range(NT // 128):
                tk.dma("sp", [], [bXS[b][t]], XS[b, t * 128:(t + 1) * 128, :], xin[b, t * 128:(t + 1) * 128, :])

        def peer_phase(l):
            with contextlib.ExitStack() as es:
                Wq = sb(es, "pr_Wq", [128, 8, 2048], BF16)
                bWq = Buf()
                load_w_bf16(es, "pr", Wq, bWq, peer_wq[l], 2048, 8)
                skT = sb(es, "pr_skT", [128, 16, 128])
                bsk = Buf()
                N = NormCtx(es, "pr")
                hT = sb(es, "pr_hT", [128, 8, 128], BF16)
                bhT = Buf()
                pQ = ps(es, "pr_pQ", [128, 2048])
                bpQ = Buf()
                qT = sb(es, "pr_qT", [128, 16, 128])
                bqT = Buf()
                S1 = sb(es, "pr_S1", [128, 16, 128])
                S2 = sb(es, "pr_S2", [128, 16, 128])
                bS1, bS2 = Buf(), Buf()
                tops = sb(es, "pr_tops", [128, 16, 16])
                topu = sb(es, "pr_topu", [128, 16, 16], U32)
                topf = sb(es, "pr_topf", [128, 16, 16])
                btop = Buf()
                cand = sb(es, "pr_cand", [128, 8, 256])
                cand2 = sb(es, "pr_cand2", [128, 8, 256])
                bcand, bcand2 = Buf(), Buf()
                best = sb(es, "pr_best", [128, 8, 16])
                posu = sb(es, "pr_posu", [128, 8, 16], U32)
                pau = sb(es, "pr_pau", [128, 8, 16], U32)
                pbu = sb(es, "pr_pbu", [128, 8, 16], U32)
                paf = sb(es, "pr_paf", [128, 8, 16])
                pbf = sb(es, "pr_pbf", [128, 8, 16])
                bbest = Buf()
                oh = sb(es, "pr_oh", [128, 8, 16, 16])
                boh = Buf()
                isel = sb(es, "pr_isel", [128, 128])
                jsel = sb(es, "pr_jsel", [128, 128])
                idxf = sb(es, "pr_idxf", [128, 128])
                idxi = sb(es, "pr_idxi", [128, 128], I32)
                bidx = Buf()
                gate = sb(es, "pr_gate", [128, 8, 16])
                gsum = sb(es, "pr_gsum", [128, 8])
                bgate = Buf()
                actv = sb(es, "pr_actv", [128, 128])
                coef = sb(es, "pr_coef", [128, 128])
                bact, bcoef = Buf(), Buf()
                NG = 6
                gbuf = [sb(es, "pr_g%d" % i, [128, D]) for i in range(NG)]
                bg = [Buf() for _ in range(NG)]
                junk2 = sb(es, "pr_junk2", [128, D])
                bj2 = Buf()
                acc = sb(es, "pr_acc", [128, D])
                bacc = Buf()
                skl = sb(es, "pr_skl", [128, 16, 128])
                tk.dma("sp", [], [bsk], skl[:], peer_sk[l].rearrange("h c k d -> k (h c) d"))
                for g4 in range(4):
                    for j in range(4):
                        hc = g4 * 4 + j
                        tk.op("pe", [bsk, bC], [bpQ], "transpose", out=pQ[:, j * 128:(j + 1) * 128], in_=skl[:, hc, :],
                              identity=ident[:])
                    tk.op("dve", [bpQ], [bqT], "tensor_copy", out=skT[:, g4 * 4:(g4 + 1) * 4, :],
                          in_=pQ[:, 0:512].rearrange("p (a b) -> p a b", b=128))
                cur_n = None
                modt = None
                for b in range(NB):
                    for (t, n, isc) in seg_tiles(b):
                        if isc and l == depth - 1:
                            continue
                        if n != cur_n:
                            cur_n = n
                            if modt is None:
                                bm = Buf()
                                modt = [sb(es, "pr_mod%d" % j, [128, D]) for j in range(3)]
                            for j, w in enumerate((4, 3, 5)):
                                tk.dma("sp", [bMOD], [bm], modt[j][:], MOD[l, n:n + 1, w * D:(w + 1) * D].to_broadcast([128, D]))
                        i = 0
                        tk.dma("sp", [bXS[b][t]], [N.bxt[i]], N.xt[i][:], XS[b, t * 128:(t + 1) * 128, :])
                        norm_mod_T(N, i, modt[0], modt[1], bm, hT[:], bhT)
                        for g4 in range(4):
                            for j in range(4):
                                ch = g4 * 4 + j
                                for k in range(8):
                                    tk.op("pe", [bhT, bWq], [bpQ], "matmul", pQ[:, j * 128:(j + 1) * 128],
                                          lhsT=Wq[:, k, ch * 128:(ch + 1) * 128], rhs=hT[:, k, :], start=(k == 0), stop=(k == 7))
                            tk.op("act", [bpQ], [bqT], "activation", out=qT[:, g4 * 4:(g4 + 1) * 4, :],
                                  in_=pQ[:, 0:512].rearrange("p (a b) -> p a b", b=128), func=AF.Copy)
                        for hc in range(16):
                            tk.op("pe", [bqT], [bpQ], "matmul", pQ[:, hc * 128:(hc + 1) * 128], lhsT=qT[:, hc, :],
                                  rhs=skT[:, hc, :], start=True, stop=True)
                        tk.op("dve", [bpQ], [bS1], "tensor_copy", out=S1[:], in_=pQ[:].rearrange("p (a b) -> p a b", b=128))
                        for hc in range(16):
                            tk.op("dve", [bS1], [btop], "max", out=tops[:, hc, 0:8], in_=S1[:, hc, :])
                            tk.op("dve", [bS1, btop], [btop], "max_index", out=topu[:, hc, 0:8], in_max=tops[:, hc, 0:8],
                                  in_values=S1[:, hc, :])
                            tk.op("dve", [bS1, btop], [bS2], "match_replace", out=S2[:, hc, :], in_to_replace=tops[:, hc, 0:8],
                                  in_values=S1[:, hc, :], imm_value=-1e30)
                            tk.op("dve", [bS2], [btop], "max", out=tops[:, hc, 8:16], in_=S2[:, hc, :])
                            tk.op("dve", [bS2, btop], [btop], "max_index", out=topu[:, hc, 8:16], in_max=tops[:, hc, 8:16],
                                  in_values=S2[:, hc, :])
                        tk.op("dve", [btop], [btop], "tensor_copy", out=topf[:], in_=topu[:])
                        tb_ = tops[:]
                        in0 = bc_ap(tb_, [(32, 8), (1, 16), (0, 16)])
                        in1 = bass.AP(tb_.tensor, tb_.offset + 16, [list(tb_.ap[0]), [32, 8], [0, 16], [1, 16]])
                        tk.op("dve", [btop], [bcand], "tensor_tensor", out=cand[:].rearrange("p h (a b) -> p h a b", b=16),
                              in0=in0, in1=in1, op=ALU.add)
                        for h in range(8):
                            tk.op("dve", [bcand], [bbest], "max", out=best[:, h, 0:8], in_=cand[:, h, :])
                            tk.op("dve", [bcand, bbest], [bbest], "max_index", out=posu[:, h, 0:8], in_max=best[:, h, 0:8],
                                  in_values=cand[:, h, :])
                            tk.op("dve", [bcand, bbest], [bcand2], "match_replace", out=cand2[:, h, :],
                                  in_to_replace=best[:, h, 0:8], in_values=cand[:, h, :], imm_value=-1e30)
                            tk.op("dve", [bcand2], [bbest], "max", out=best[:, h, 8:16], in_=cand2[:, h, :])
                            tk.op("dve", [bcand2, bbest], [bbest], "max_index", out=posu[:, h, 8:16], in_max=best[:, h, 8:16],
                                  in_values=cand2[:, h, :])
                        tk.op("dve", [bbest], [bbest], "tensor_single_scalar", out=pau[:], in_=posu[:], scalar=4,
                              op=ALU.logical_shift_right)
                        tk.op("dve", [bbest], [bbest], "tensor_single_scalar", out=pbu[:], in_=posu[:], scalar=15,
                              op=ALU.bitwise_and)
                        tk.op("dve", [bbest], [bbest], "tensor_copy", out=paf[:], in_=pau[:])
                        tk.op("dve", [bbest], [bbest], "tensor_copy", out=pbf[:], in_=pbu[:])
                        io = iota16[:]
                        iob = bc_ap(io, [(0, 8), (0, 16), (1, 16)])
                        tf = topf[:]
                        for which, pf, dst in ((0, paf, isel), (1, pbf, jsel)):
                            pfb = bc_ap(pf[:], [(16, 8), (1, 16), (0, 16)])
                            tk.op("dve", [bbest, bC], [boh], "tensor_tensor", out=oh[:], in0=iob, in1=pfb, op=ALU.is_equal)
                            tfb = bass.AP(tf.tensor, tf.offset + 16 * which, [list(tf.ap[0]), [32, 8], [0, 16], [1, 16]])
                            tk.op("dve", [boh, btop], [boh], "tensor_tensor", out=oh[:], in0=oh[:], in1=tfb, op=ALU.mult)
                            tk.op("dve", [boh], [bidx], "tensor_reduce", out=dst[:].rearrange("p (h k) -> p h k", k=16),
                                  in_=oh[:], axis=AX.X, op=ALU.add)
                        tk.op("dve", [bidx], [bidx], "scalar_tensor_tensor", out=idxf[:], in0=isel[:], scalar=128.0, in1=jsel[:],
                              op0=ALU.mult, op1=ALU.add)
                        tk.op("dve", [bidx], [bidx], "tensor_copy", out=idxi[:], in_=idxf[:])
                        bs_ = best[:]
                        mxb = bc_ap(bs_, [(16, 8), (0, 16)])
                        tk.op("dve", [bbest], [bgate], "tensor_tensor", out=gate[:], in0=best[:], in1=mxb, op=ALU.subtract)
                        tk.op("act", [bgate], [bgate], "activation", out=gate[:], in_=gate[:], func=AF.Exp)
                        tk.op("dve", [bgate], [bgate], "tensor_reduce", out=gsum[:], in_=gate[:], axis=AX.X, op=ALU.add)
                        tk.op("dve", [bgate], [bgate], "reciprocal", out=gsum[:], in_=gsum[:])
                        tk.op("dve", [bgate], [bgate], "tensor_tensor", out=gate[:], in0=gate[:],
                              in1=bc_ap(gsum[:], [(1, 8), (0, 16)]), op=ALU.mult)
                        if os.environ.get("PEERDBG"):
                            if not isc:
                                tl = t - CL // 128
                                tk.dma("sp", [bidx], [bOUT], yout[b, tl * 128:(tl + 1) * 128, 0:128], idxf[:])
                                tk.dma("sp", [bgate], [bOUT], yout[b, tl * 128:(tl + 1) * 128, 128:256], gate[:].rearrange("p h k -> p (h k)"))
                                tk.dma("sp", [bbest], [bOUT], yout[b, tl * 128:(tl + 1) * 128, 256:384], best[:].rearrange("p h k -> p (h k)"))
                                tk.dma("sp", [btop], [bOUT], yout[b, tl * 128:(tl + 1) * 128, 384:640], topf[:].rearrange("p h k -> p (h k)"))
                                tk.dma("sp", [btop], [bOUT], yout[b, tl * 128:(tl + 1) * 128, 640:896], tops[:].rearrange("p h k -> p (h k)"))
                            continue
                        gi = 0
                        for s in range(128):
                            j = gi % NG
                            gi += 1
                            tk.dma("pool", [bidx], [bg[j]], gbuf[j][:], peer_u.rearrange("l e d -> (l e) d"),
                                   indirect=bass.IndirectOffsetOnAxis(ap=idxi[:, s:s + 1], axis=0), element_offset=l * 16384 * D)
                            tk.op("dve", [bg[j], N.bh], [bj2, bact], "scalar_tensor_tensor", out=junk2[:], in0=gbuf[j][:], scalar=1.0,
                                  in1=N.h[:], op0=ALU.mult, op1=ALU.mult, accum_out=actv[:, s:s + 1])
                        tk.op("act", [bact], [bcoef], "activation", out=coef[:], in_=actv[:], func=AF.Gelu)
                        tk.op("dve", [bcoef, bgate], [bcoef], "tensor_tensor", out=coef[:], in0=coef[:],
                              in1=gate[:].rearrange("p h k -> p (h k)"), op=ALU.mult)
                        for s in range(128):
                            j = gi % NG
                            gi += 1
                            tk.dma("pool", [bidx], [bg[j]], gbuf[j][:], peer_v.rearrange("l e d -> (l e) d"),
                                   indirect=bass.IndirectOffsetOnAxis(ap=idxi[:, s:s + 1], axis=0), element_offset=l * 16384 * D)
                            if s == 0:
                                tk.op("dve", [bg[j], bcoef], [bacc], "tensor_scalar", out=acc[:], in0=gbuf[j][:],
                                      scalar1=coef[:, 0:1], scalar2=None, op0=ALU.mult)
                            else:
                                tk.op("dve", [bg[j], bcoef, bacc], [bacc], "scalar_tensor_tensor", out=acc[:], in0=gbuf[j][:],
                                      scalar=coef[:, s:s + 1], in1=acc[:], op0=ALU.mult, op1=ALU.add)
                        tk.op("dve", [bacc, bm], [bacc], "tensor_tensor", out=acc[:], in0=acc[:], in1=modt[2][:], op=ALU.mult)
                        tk.op("dve", [bacc, N.bxt[i]], [bacc], "tensor_tensor", out=acc[:], in0=acc[:], in1=N.xt[i][:], op=ALU.add)
                        tk.dma("sp", [bacc], [bXS[b][t]], XS[b, t * 128:(t + 1) * 128, :], acc[:])
                tk.barrier()

        def dn_phase(l, di):
            last = (l == depth - 1)
            CT = CL // 128
            segs = lambda b: ((0, CL, 2), (CL, T, b))
            with contextlib.ExitStack() as es:
                SEG = max(T, CL)
                Wg = sb(es, "dn_Wg", [128, 8, 64], BF16)
                bWg = Buf()
                load_w_bf16(es, "dng", Wg, bWg, dn_w_in[di][:, 6144:6208], 64, 8)
                cwT = sb(es, "dn_cwT", [128, 32, 5])
                bcw = Buf()
                for cc in range(32):
                    tk.dma("sp", [], [bcw], cwT[:, cc, :], dn_conv_w[di][:, cc * 128:(cc + 1) * 128].rearrange("t c -> c t"),
                           allow_slow_non_contiguous=True)
                alog, bal = load_bc(es, "dn_alog", dn_a_log[di:di + 1, :])
                dtb, bdt = load_bc(es, "dn_dtb", dn_dt_bias[di:di + 1, :])
                tk.op("act", [bal], [bal], "activation", out=alog[:], in_=alog[:], func=AF.Exp)
                tk.op("dve", [bal], [bal], "tensor_scalar", out=alog[:], in0=alog[:], scalar1=-1.0, scalar2=None, op0=ALU.mult)
                hTs = sb(es, "dn_hTs", [128, 8, SEG], BF16)
                bhTs = Buf()
                N = NormCtx(es, "dn")
                modt = [sb(es, "dn_mod%d" % j, [128, D]) for j in range(2)]
                bm = Buf()
                pG = ps(es, "dn_pG", [128, 64])
                bpG = Buf()
                gsb = sb(es, "dn_gsb", [128, 64])
                gbt = sb(es, "dn_gbt", [128, 64])
                gx = sb(es, "dn_gx", [128, 4, 32])
                bgs = Buf()
                Wc = [sb(es, "dn_Wc%d" % i, [128, 8, 128]) for i in range(2)]
                bWc = [Buf(), Buf()]
                Wcb = sb(es, "dn_Wcb", [128, 8, 128], BF16)
                bWcb = Buf()
                projT = sb(es, "dn_projT", [128, SEG + 4])
                acc = sb(es, "dn_acc", [128, SEG])
                bproj, bacc = Buf(), Buf()
                pJ = [ps(es, "dn_pJ%d" % i, [128, 512]) for i in range(2)]
                bpJ = [Buf(), Buf()]
                pTv = ps(es, "dn_pTv", [128, 4, 128])
                bpTv = Buf()
                vtm = sb(es, "dn_vtm", [128, 4, 128])
                bvtm = Buf()
                for b in range(NB):
                    for (tok0, ln, n) in segs(b):
                        for j, w in enumerate((1, 0)):
                            tk.dma("sp", [bMOD], [bm], modt[j][:], MOD[l, n:n + 1, w * D:(w + 1) * D].to_broadcast([128, D]))
                        for tt in range(ln // 128):
                            t = tok0 // 128 + tt
                            tk.dma("sp", [bXS[b][t]], [N.bxt[0]], N.xt[0][:], XS[b, t * 128:(t + 1) * 128, :])
                            norm_mod_T(N, 0, modt[0], modt[1], bm, hTs[:, :, tt * 128:(tt + 1) * 128], bhTs)
                            for k in range(8):
                                tk.op("pe", [bhTs, bWg], [bpG], "matmul", pG[:], lhsT=hTs[:, k, tt * 128:(tt + 1) * 128], rhs=Wg[:, k, :],
                                      start=(k == 0), stop=(k == 7))
                            tk.op("dve", [bpG], [bgs], "tensor_copy", out=gsb[:], in_=pG[:])
                            g4 = gsb[:].rearrange("p (d k h) -> p d k h", d=2, k=2)
                            o4 = gbt[:].rearrange("p (d k h) -> p d k h", d=2, k=2)
                            x3 = gx[:, 0, :].rearrange("p (d h) -> p d h", d=2)
                            tk.op("act", [bgs], [bgs], "activation", out=o4[:, :, 0, :], in_=g4[:, :, 0, :], func=AF.Sigmoid)
                            tk.op("dve", [bgs, bdt], [bgs], "tensor_tensor", out=x3, in0=g4[:, :, 1, :],
                                  in1=dtb[:].rearrange("p (d h) -> p d h", d=2), op=ALU.add)
                            tk.op("dve", [bgs], [bgs], "tensor_scalar", out=gx[:, 1, :], in0=gx[:, 0, :], scalar1=-1.0, scalar2=None, op0=ALU.mult)
                            tk.op("dve", [bgs], [bgs], "tensor_tensor", out=gx[:, 1, :], in0=gx[:, 1, :], in1=gx[:, 0, :], op=ALU.max)
                            tk.op("act", [bgs], [bgs], "activation", out=gx[:, 1, :], in_=gx[:, 1, :], func=AF.Exp, scale=-1.0)
                            tk.op("act", [bgs], [bgs], "activation", out=gx[:, 1, :], in_=gx[:, 1, :], func=AF.Ln, bias=1.0)
                            tk.op("dve", [bgs], [bgs], "tensor_scalar", out=gx[:, 2, :], in0=gx[:, 0, :], scalar1=0.0, scalar2=None, op0=ALU.max)
                            tk.op("dve", [bgs], [bgs], "tensor_tensor", out=gx[:, 2, :], in0=gx[:, 2, :], in1=gx[:, 1, :], op=ALU.add)
                            tk.op("dve", [bgs, bal], [bgs], "tensor_tensor", out=o4[:, :, 1, :], in0=gx[:, 2, :].rearrange("p (d h) -> p d h", d=2),
                                  in1=alog[:].rearrange("p (d h) -> p d h", d=2), op=ALU.mult)
                            tk.dma("pool", [bgs], [bGB], GB[b, t * 128:(t + 1) * 128, :], gbt[:])
                        nblk = [(c0, min(512, ln - c0)) for c0 in range(0, ln, 512)]
                        for cc in range(48):
                            i = cc % 2
                            tk.dma("sp", [], [bWc[i]], Wc[i][:], dn_w_in[di][:, cc * 128:(cc + 1) * 128].rearrange("(k p) c -> p k c", p=128))
                            tk.op("dve", [bWc[i]], [bWcb], "tensor_copy", out=Wcb[:], in_=Wc[i][:])
                            if cc < 32:
                                tk.op("dve", [], [bproj], "memset", projT[:, 0:2], 0.0)
                                tk.op("dve", [], [bproj], "memset", projT[:, ln + 2:ln + 4], 0.0)
                            dst, bdst, off = (projT, bproj, 2) if cc < 32 else (acc, bacc, 0)
                            for bi, (c0, bw) in enumerate(nblk):
                                j = bi % 2
                                for k in range(8):
                                    tk.op("pe", [bhTs, bWcb], [bpJ[j]], "matmul", pJ[j][:, 0:bw], lhsT=Wcb[:, k, :], rhs=hTs[:, k, c0:c0 + bw],
                                          start=(k == 0), stop=(k == 7))
                                tk.op("act", [bpJ[j]], [bdst], "activation", out=dst[:, off + c0:off + c0 + bw], in_=pJ[j][:, 0:bw], func=AF.Copy)
                            if cc < 32:
                                tk.op("dve", [bproj, bcw], [bacc], "tensor_scalar", out=acc[:, 0:ln], in0=projT[:, 0:ln], scalar1=cwT[:, cc, 0:1],
                                      scalar2=None, op0=ALU.mult)
                                for tp in range(1, 5):
                                    tk.op("dve", [bproj, bcw, bacc], [bacc], "scalar_tensor_tensor", out=acc[:, 0:ln], in0=projT[:, tp:tp + ln],
                                          scalar=cwT[:, cc, tp:tp + 1], in1=acc[:, 0:ln], op0=ALU.mult, op1=ALU.add)
                            tk.op("act", [bacc], [bacc], "activation", out=acc[:, 0:ln], in_=acc[:, 0:ln], func=AF.Silu)
                            if cc < 16:
                                tk.op("dve", [bacc], [bproj], "tensor_tensor", out=projT[:, 0:ln], in0=acc[:, 0:ln], in1=acc[:, 0:ln], op=ALU.mult)
                                for bi, (c0, bw) in enumerate(nblk):
                                    j = bi % 2
                                    tk.op("pe", [bproj, bC], [bpJ[j]], "matmul", pJ[j][:, 0:bw], lhsT=ones[:], rhs=projT[:, c0:c0 + bw], start=True, stop=True)
                                    tk.op("act", [bpJ[j]], [bproj], "activation", out=projT[:, c0:c0 + bw], in_=pJ[j][:, 0:bw], func=AF.Sqrt, bias=EPS)
                                tk.op("dve", [bproj], [bproj], "reciprocal", out=projT[:, 0:ln], in_=projT[:, 0:ln])
                                tk.op("dve", [bproj, bacc], [bacc], "scalar_tensor_tensor", out=acc[:, 0:ln], in0=acc[:, 0:ln],
                                      scalar=(128 ** -0.5 if cc < 8 else 1.0), in1=projT[:, 0:ln], op0=ALU.mult, op1=ALU.mult)
                                tk.dma("pool", [bacc], [bQKT], QKT[b, :, cc, tok0:tok0 + ln], acc[:, 0:ln])
                            else:
                                dd, bdd, c16 = (VV, bVV, cc - 16) if cc < 32 else (ZZ, bZZ, cc - 32)
                                for t4 in range(0, ln // 128, 4):
                                    nt4 = min(4, ln // 128 - t4)
                                    for j in range(nt4):
                                        tk.op("pe", [bacc, bC], [bpTv], "transpose", out=pTv[:, j, :], in_=acc[:, (t4 + j) * 128:(t4 + j + 1) * 128],
                                              identity=ident[:])
                                    tk.op("dve", [bpTv], [bvtm], "tensor_copy", out=vtm[:, 0:nt4, :], in_=pTv[:, 0:nt4, :])
                                    r0 = tok0 + t4 * 128
                                    tk.dma("pool", [bvtm], [bdd], dd[b, r0:r0 + nt4 * 128, c16 * 128:(c16 + 1) * 128].rearrange("(j p) c -> p j c", p=128),
                                           vtm[:, 0:nt4, :])
                tk.barrier()
            with contextlib.ExitStack() as es:
                C = 64
                S32 = sb(es, "ds_S", [128, 16, 128])
                bS = Buf()
                qk = sb(es, "ds_qk", [128, 16, C])
                bqk = Buf()
                vsb = sb(es, "ds_v", [C, 16, 128])
                bv = Buf()
                gb = sb(es, "ds_gb", [C, 64])
                bgb = Buf()
                PA = ps(es, "ds_PA", [128, 2048])
                PB = ps(es, "ds_PB", [128, 2048])
                bPA, bPB = Buf(), Buf()
                sm = sb(es, "ds_sm", [128, 8, 16])
                bsm = Buf()
                ktm = sb(es, "ds_ktm", [C, 8, 128])
                bktm = Buf()
                dg = sb(es, "ds_dg", [C, 32, C])
                bdg = Buf()
                dec = sb(es, "ds_dec", [C, 16, C])
                bdec = Buf()
                U = [sb(es, "ds_U%d" % i, [C, 16, C]) for i in range(6)]
                bU = [Buf() for _ in range(6)]
                Lm = [sb(es, "ds_L%d" % i, [C, 16, C]) for i in range(2)]
                bL = [Buf(), Buf()]
                qkm = sb(es, "ds_qkm", [C, 16, C])
                bqkm = Buf()
                yv = [sb(es, "ds_y%d" % i, [C, 16, 128]) for i in range(2)]
                by = [Buf(), Buf()]
                tmpv = sb(es, "ds_tmpv", [C, 16, 128])
                btmp = Buf()
                osb = sb(es, "ds_o", [C, 16, 128])
                bo = Buf()

                def P3(Pt, n, w):
                    return Pt[0:C, 0:n * w].rearrange("p (h c) -> p h c", c=w)

                for b in range(NB):
                    for d in range(2):
                        mk = masks[:, 3 * d:3 * d + 3, :]
                        tk.op("dve", [], [bS], "memset", S32[:], 0.0)
                        order = []
                        for (tok0, ln, n) in segs(b):
                            cs = list(range(tok0, tok0 + ln, C))
                            order += cs if d == 0 else cs[::-1]
                        for t0 in order:
                            if last and t0 < CL:
                                pass
                            tk.dma("sp", [bQKT], [bqk], qk[:], QKT[b, :, :, t0:t0 + C])
                            tk.dma("sp", [bVV], [bv], vsb[:], VV[b, t0:t0 + C, :].rearrange("p (h c) -> p h c", c=128))
                            tk.dma("sp", [bGB], [bgb], gb[:], GB[b, t0:t0 + C, :])
                            beta = gb[:, d * 32:d * 32 + 16]
                            gg = gb[:, d * 32 + 16:d * 32 + 32]
                            tk.op("pe", [bC, bgb], [bPB], "matmul", PB[0:C, 0:16], lhsT=mk[:, 0, :], rhs=gg, start=True, stop=True)
                            tk.op("pe", [bC, bgb], [bPB], "matmul", PB[:, 16:32], lhsT=ones[0:C, :], rhs=gg, start=True, stop=True)
                            tk.op("dve", [bPB], [bsm], "tensor_copy", out=sm[0:C, 0, :], in_=PB[0:C, 0:16])
                            tk.op("act", [bPB], [bsm], "activation", out=sm[0:C, 1, :], in_=PB[0:C, 0:16], func=AF.Exp)
                            tk.op("act", [bPB], [bsm], "activation", out=sm[:, 4, :], in_=PB[:, 16:32], func=AF.Exp)
                            tk.op("dve", [bPB, bsm], [bsm], "tensor_tensor", out=sm[0:C, 5, :], in0=PB[0:C, 16:32], in1=sm[0:C, 0, :], op=ALU.subtract)
                            tk.op("act", [bsm], [bsm], "activation", out=sm[0:C, 2, :], in_=sm[0:C, 5, :], func=AF.Exp)
                            tk.op("dve", [bsm, bgb], [bsm], "tensor_tensor", out=sm[0:C, 3, :], in0=sm[0:C, 1, :], in1=beta, op=ALU.mult)
                            for hk in range(8):
                                tk.op("pe", [bqk, bC], [bPB], "transpose", out=PB[0:C, 512 + hk * 128:512 + (hk + 1) * 128], in_=qk[:, 8 + hk, :],
                                      identity=ident[:])
                            tk.op("act", [bPB], [bktm], "activation", out=ktm[:], in_=PB[0:C, 512:1536].rearrange("p (h c) -> p h c", c=128), func=AF.Copy)
                            idb = bc_ap(ident[0:C, 0:C], [(0, 16), (1, C)])
                            tk.op("dve", [bC, bsm], [bdg], "tensor_tensor", out=dg[:, 0:16, :], in0=idb, in1=bc_ap(sm[0:C, 0, :], [(1, 16), (0, C)]), op=ALU.mult)
                            tk.op("dve", [bC, bgb], [bdg], "tensor_tensor", out=dg[:, 16:32, :], in0=idb, in1=bc_ap(beta, [(1, 16), (0, C)]), op=ALU.mult)
                            for q4 in range(4):
                                tk.op("pe", [bdg, bC], [bPA], "matmul", PA[0:C, q4 * 512:(q4 + 1) * 512], lhsT=ones[0:C, 0:C],
                                      rhs=dg[:, q4 * 8:(q4 + 1) * 8, :].rearrange("p a b -> p (a b)"), start=True, stop=True)
                            gcb = P3(PA, 16, C)
                            btb = PA[0:C, 1024:2048].rearrange("p (h c) -> p h c", c=C)
                            tk.op("dve", [bPA, bsm], [bdec], "tensor_tensor", out=dec[:], in0=gcb, in1=bc_ap(sm[0:C, 0, :], [(1, 16), (0, C)]), op=ALU.subtract)
                            tk.op("dve", [bdec], [bdec], "tensor_scalar", out=dec[:], in0=dec[:], scalar1=0.0, scalar2=None, op0=ALU.min)
                            tk.op("act", [bdec], [bdec], "activation", out=dec[:], in_=dec[:], func=AF.Exp)
                            for hk in range(8):
                                tk.op("pe", [bqk], [bPB], "matmul", PB[0:C, hk * C:(hk + 1) * C], lhsT=qk[:, 8 + hk, :], rhs=qk[:, 8 + hk, :], start=True, stop=True)
                                tk.op("pe", [bqk], [bPB], "matmul", PB[0:C, 512 + hk * C:512 + (hk + 1) * C], lhsT=qk[:, 8 + hk, :], rhs=qk[:, hk, :], start=True, stop=True)
                            msk_s = bc_ap(mk[:, 2, :], [(0, 16), (1, C)])
                            msk_i = bc_ap(mk[:, 1, :], [(0, 16), (1, C)])
                            kkb = bass.AP(PB[:].tensor, PB[0:C, 0:512].offset, [list(PB[0:C, 0:512].ap[0]), [C, 8], [0, 2], [1, C]])
                            ptb = bass.AP(PB[:].tensor, PB[0:C, 512:1024].offset, [list(PB[0:C, 512:1024].ap[0]), [C, 8], [0, 2], [1, C]])
                            U0 = U[0]
                            tk.op("dve", [bdec, bPA], [bU[0]], "tensor_tensor", out=U0[:], in0=dec[:], in1=btb, op=ALU.mult)
                            tk.op("dve", [bU[0], bC], [bU[0]], "tensor_tensor", out=U0[:], in0=U0[:], in1=msk_s, op=ALU.mult)
                            tk.op("dve", [bU[0], bPB], [bU[0]], "tensor_tensor", out=U0[:].rearrange("p (a r) c -> p a r c", r=2),
                                  in0=U0[:].rearrange("p (a r) c -> p a r c", r=2), in1=kkb, op=ALU.mult)
                            tk.op("dve", [bdec, bC], [bqkm], "tensor_tensor", out=qkm[:], in0=dec[:], in1=msk_i, op=ALU.mult)
                            tk.op("dve", [bqkm, bPB], [bqkm], "tensor_tensor", out=qkm[:].rearrange("p (a r) c -> p a r c", r=2),
                                  in0=qkm[:].rearrange("p (a r) c -> p a r c", r=2), in1=ptb, op=ALU.mult)
                            for h in range(16):
                                tk.op("pe", [bU[0], bC], [bPA], "transpose", out=PA[0:C, h * C:(h + 1) * C], in_=U0[:, h, :], identity=ident[0:C, 0:C])
                            tk.op("act", [bPA], [bL[0]], "activation", out=Lm[0][:], in_=P3(PA, 16, C), func=AF.Copy)
                            Pcur, bPcur, Pnxt, bPnxt = PB, bPB, PA, bPA
                            for p in range(5):
                                Lc, Uc = Lm[p % 2], U[p]
                                for h in range(16):
                                    tk.op("pe", [bL[p % 2], bU[p]], [bPcur], "matmul", Pcur[0:C, h * C:(h + 1) * C], lhsT=Uc[:, h, :], rhs=Lc[:, h, :], start=True, stop=True)
                                    tk.op("pe", [bL[p % 2], bU[p]], [bPcur], "matmul", Pcur[0:C, 1024 + h * C:1024 + (h + 1) * C], lhsT=Lc[:, h, :], rhs=Uc[:, h, :], start=True, stop=True)
                                tk.op("act", [bPcur], [bL[(p + 1) % 2]], "activation", out=Lm[(p + 1) % 2][:], in_=P3(Pcur, 16, C), func=AF.Copy)
                                tk.op("dve", [bPcur], [bU[p + 1]], "tensor_copy", out=U[p + 1][:], in_=Pcur[0:C, 1024:2048].rearrange("p (h c) -> p h c", c=C))
                                Pcur, bPcur, Pnxt, bPnxt = Pnxt, bPnxt, Pcur, bPcur
                            for h in range(16):
                                tk.op("pe", [bqk, bS], [bPcur], "matmul", Pcur[0:C, h * 128:(h + 1) * 128], lhsT=qk[:, 8 + h // 2, :], rhs=S32[:, h, :], start=True, stop=True)
                            tk.op("dve", [bPcur, bsm], [btmp], "tensor_tensor", out=tmpv[:], in0=P3(Pcur, 16, 128), in1=bc_ap(sm[0:C, 3, :], [(1, 16), (0, 128)]), op=ALU.mult)
                            tk.op("dve", [bv, bgb], [by[0]], "tensor_tensor", out=yv[0][:], in0=vsb[:], in1=bc_ap(beta, [(1, 16), (0, 128)]), op=ALU.mult)
                            tk.op("dve", [by[0], btmp], [by[0]], "tensor_tensor", out=yv[0][:], in0=yv[0][:], in1=tmpv[:], op=ALU.subtract)
                            Pcur, bPcur, Pnxt, bPnxt = Pnxt, bPnxt, Pcur, bPcur
                            yi = 0
                            for p in (5, 4, 3, 2, 1, 0):
                                for h in range(16):
                                    tk.op("pe", [bU[p], by[yi]], [bPcur], "matmul", Pcur[0:C, h * 128:(h + 1) * 128], lhsT=U[p][:, h, :], rhs=yv[yi][:, h, :], start=True, stop=True)
                                tk.op("dve", [bPcur, by[yi]], [by[1 - yi]], "tensor_tensor", out=yv[1 - yi][:], in0=yv[yi][:], in1=P3(Pcur, 16, 128),
                                      op=(ALU.add if p > 0 else ALU.subtract))
                                yi = 1 - yi
                                Pcur, bPcur, Pnxt, bPnxt = Pnxt, bPnxt, Pcur, bPcur
                            vn, bvn = yv[yi], by[yi]
                            for h in range(16):
                                tk.op("pe", [bqk, bS], [bPcur], "matmul", Pcur[0:C, h * 128:(h + 1) * 128], lhsT=qk[:, h // 2, :], rhs=S32[:, h, :], start=True, stop=True)
                            tk.op("dve", [bPcur, bsm], [bo], "tensor_tensor", out=osb[:], in0=P3(Pcur, 16, 128), in1=bc_ap(sm[0:C, 1, :], [(1, 16), (0, 128)]), op=ALU.mult)
                            Pcur, bPcur, Pnxt, bPnxt = Pnxt, bPnxt, Pcur, bPcur
                            for h in range(16):
                                tk.op("pe", [bqkm, bvn], [bPcur], "matmul", Pcur[0:C, h * 128:(h + 1) * 128], lhsT=qkm[:, h, :], rhs=vn[:, h, :], start=True, stop=True)
                            tk.op("dve", [bPcur, bo], [bo], "tensor_tensor", out=osb[:], in0=osb[:], in1=P3(Pcur, 16, 128), op=ALU.add)
                            Pcur, bPcur, Pnxt, bPnxt = Pnxt, bPnxt, Pcur, bPcur
                            tk.dma("pool", [bo], [bOO], OO[d, b, t0:t0 + C, :].rearrange("p (h c) -> p h c", c=128), osb[:])
                            tk.op("dve", [bvn, bsm], [btmp], "tensor_tensor", out=tmpv[:], in0=vn[:], in1=bc_ap(sm[0:C, 2, :], [(1, 16), (0, 128)]), op=ALU.mult)
                            for h in range(16):
                                tk.op("pe", [bktm, btmp], [bPcur], "matmul", Pcur[:, h * 128:(h + 1) * 128], lhsT=ktm[:, h // 2, :], rhs=tmpv[:, h, :], start=True, stop=True)
                            tk.op("dve", [bS, bsm], [bS], "tensor_tensor", out=S32[:], in0=S32[:], in1=bc_ap(sm[:, 4, :], [(1, 16), (0, 128)]), op=ALU.mult)
                            tk.op("dve", [bS, bPcur], [bS], "tensor_tensor", out=S32[:], in0=S32[:], in1=Pcur[:, :].rearrange("p (h c) -> p h c", c=128), op=ALU.add)
                tk.barrier()
            with contextlib.ExitStack() as es:
                Wo = sb(es, "dn_Wo", [128, 16, D], BF16)
                bWo = Buf()
                load_w_bf16(es, "dno", Wo, bWo, dn_w_out[di], D, 16)
                ng_, bng = load_bc(es, "dn_ng", dn_norm_g[di:di + 1, :])
                of = sb(es, "dc_of", [128, 2048])
                ob = sb(es, "dc_ob", [128, 2048])
                zt = sb(es, "dc_z", [128, 2048])
                bof, bob, bz = Buf(), Buf(), Buf()
                sq = sb(es, "dc_sq", [128, 2048])
                ss = sb(es, "dc_ss", [128, 16])
                bsq = Buf()
                onb = sb(es, "dc_onb", [128, 2048], BF16)
                bonb = Buf()
                xt = sb(es, "dc_x", [128, D])
                bx = Buf()
                pT = ps(es, "dc_pT", [128, 16, 128], BF16)
                bpT = Buf()
                oT = sb(es, "dc_oT", [128, 16, 128], BF16)
                boT = Buf()
                pY = ps(es, "dc_pY", [128, D])
                bpY = Buf()
                yt = sb(es, "dc_y", [128, D])
                by_ = Buf()
                g1t = sb(es, "dc_g1", [128, D])
                bg1 = Buf()
                cur_n = None
                for b in range(NB):
                    for (t, n, isc) in seg_tiles(b):
                        if isc and last:
                            continue
                        if n != cur_n:
                            cur_n = n
                            tk.dma("sp", [bMOD], [bg1], g1t[:], MOD[l, n:n + 1, 2 * D:3 * D].to_broadcast([128, D]))
                        rs = slice(t * 128, (t + 1) * 128)
                        tk.dma("sp", [bOO], [bof], of[:], OO[0, b, rs, :])
                        tk.dma("sp", [bOO], [bob], ob[:], OO[1, b, rs, :])
                        tk.dma("sp", [bZZ], [bz], zt[:], ZZ[b, rs, :])
                        tk.dma("sp", [bXS[b][t]], [bx], xt[:], XS[b, rs, :])
                        tk.op("dve", [bof, bob], [bof], "tensor_tensor", out=of[:], in0=of[:], in1=ob[:], op=ALU.add)
                        tk.op("dve", [bof], [bsq], "tensor_tensor", out=sq[:], in0=of[:], in1=of[:], op=ALU.mult)
                        tk.op("dve", [bsq], [bsq], "tensor_reduce", out=ss[:], in_=sq[:].rearrange("p (h c) -> p h c", c=128), axis=AX.X, op=ALU.add)
                        tk.op("act", [bsq], [bsq], "activation", out=ss[:], in_=ss[:], func=AF.Sqrt, bias=EPS, scale=1.0 / 128)
                        tk.op("dve", [bsq], [bsq], "reciprocal", out=ss[:], in_=ss[:])
                        o3 = of[:].rearrange("p (h c) -> p h c", c=128)
                        tk.op("dve", [bof, bsq], [bof], "tensor_tensor", out=o3, in0=o3, in1=bc_ap(ss[:], [(1, 16), (0, 128)]), op=ALU.mult)
                        tk.op("dve", [bof, bng], [bof], "tensor_tensor", out=o3, in0=o3, in1=bc_ap(ng_[:], [(0, 16), (1, 128)]), op=ALU.mult)
                        tk.op("dve", [bof, bz], [bonb], "tensor_tensor", out=onb[:], in0=of[:], in1=zt[:], op=ALU.mult)
                        for k in range(16):
                            tk.op("pe", [bonb, bC], [bpT], "transpose", out=pT[:, k, :], in_=onb[:, k * 128:(k + 1) * 128], identity=identb[:])
                        tk.op("act", [bpT], [boT], "activation", out=oT[:], in_=pT[:], func=AF.Copy)
                        for cg in range(2):
                            for k in range(16):
                                tk.op("pe", [boT, bWo], [bpY], "matmul", pY[:, cg * 512:(cg + 1) * 512], lhsT=oT[:, k, :],
                                      rhs=Wo[:, k, cg * 512:(cg + 1) * 512], start=(k == 0), stop=(k == 15))
                        tk.op("dve", [bpY, bg1], [by_], "tensor_tensor", out=yt[:], in0=pY[:], in1=g1t[:], op=ALU.mult)
                        tk.op("dve", [by_, bx], [by_], "tensor_tensor", out=yt[:], in0=yt[:], in1=xt[:], op=ALU.add)
                        tk.dma("pool", [by_], [bXS[b][t]], XS[b, rs, :], yt[:])
                tk.barrier()

        def att_phase(l, ai):
            last = (l == depth - 1)
            NTT = NT // 128
            CT = CL // 128
            with contextlib.ExitStack() as es:
                Win = sb(es, "at_Win", [128, 8, 1536], BF16)
                bWin = Buf()
                load_w_bf16(es, "at", Win, bWin, att_w_in[ai], 1536, 8)
                gqk = sb(es, "at_gqk", [128, 10, 128])
                bg_ = Buf()
                for hh in range(10):
                    src = (att_qn_g if hh < 8 else att_kn_g)[ai:ai + 1, :]
                    tk.dma("sp", [], [bg_], gqk[:, hh, :], src.to_broadcast([128, 128]))
                N = NormCtx(es, "at")
                hT = sb(es, "at_hT", [128, 8, 128], BF16)
                bhT = Buf()
                pP = ps(es, "at_pP", [128, 1536])
                bpP = Buf()
                p_sb = sb(es, "at_p", [128, 1536])
                bp = Buf()
                sq = sb(es, "at_sq", [128, 1280])
                ss = sb(es, "at_ss", [128, 10])
                bsq = Buf()
                rope = sb(es, "at_rope", [128, 2, 64])
                brope = Buf()
                ra = sb(es, "at_ra", [128, 10, 64])
                rb = sb(es, "at_rb", [128, 10, 64])
                rr = sb(es, "at_rr", [128, 1280])
                brr = Buf()
                qkb = sb(es, "at_qkb", [128, 1280], BF16)
                vb = sb(es, "at_vb", [128, 256], BF16)
                bqkb, bvb = Buf(), Buf()
                pT2 = ps(es, "at_pT2", [128, 10, 128], BF16)
                bpT2 = Buf()
                qkT = sb(es, "at_qkT", [128, 10, 128], BF16)
                bqkT = Buf()
                modt = [sb(es, "at_mod%d" % j, [128, D]) for j in range(2)]
                bm = Buf()
                cur_n = None
                for b in range(NB):
                    for (t, n, isc) in seg_tiles(b):
                        if n != cur_n:
                            cur_n = n
                            for j, w in enumerate((1, 0)):
                                tk.dma("sp", [bMOD], [bm], modt[j][:], MOD[l, n:n + 1, w * D:(w + 1) * D].to_broadcast([128, D]))
                        tk.dma("sp", [bXS[b][t]], [N.bxt[0]], N.xt[0][:], XS[b, t * 128:(t + 1) * 128, :])
                        norm_mod_T(N, 0, modt[0], modt[1], bm, hT[:], bhT)
                        for cg in range(3):
                            for k in range(8):
                                tk.op("pe", [bhT, bWin], [bpP], "matmul", pP[:, cg * 512:(cg + 1) * 512], lhsT=hT[:, k, :],
                                      rhs=Win[:, k, cg * 512:(cg + 1) * 512], start=(k == 0), stop=(k == 7))
                        tk.op("dve", [bpP], [bp], "tensor_copy", out=p_sb[:], in_=pP[:])
                        tk.op("dve", [bp], [bsq], "tensor_tensor", out=sq[:], in0=p_sb[:, 0:1280], in1=p_sb[:, 0:1280], op=ALU.mult)
                        tk.op("dve", [bsq], [bsq], "tensor_reduce", out=ss[:], in_=sq[:].rearrange("p (h d) -> p h d", d=128),
                              axis=AX.X, op=ALU.add)
                        tk.op("act", [bsq], [bsq], "activation", out=ss[:], in_=ss[:], func=AF.Sqrt, bias=EPS, scale=1.0 / 128)
                        tk.op("dve", [bsq], [bsq], "reciprocal", out=ss[:], in_=ss[:])
                        sq3 = sq[:].rearrange("p (h d) -> p h d", d=128)
                        tk.op("dve", [bp, bsq], [bsq], "tensor_tensor", out=sq3, in0=p_sb[:, 0:1280].rearrange("p (h d) -> p h d", d=128),
                              in1=bc_ap(ss[:], [(1, 10), (0, 128)]), op=ALU.mult)
                        tk.op("dve", [bsq, bg_], [bsq], "tensor_tensor", out=sq3, in0=sq3, in1=gqk[:], op=ALU.mult)
                        if not isc:
                            tl = t - CT
                            tk.dma("sp", [], [brope], rope[:], c_rope[tl * 128:(tl + 1) * 128])
                            s4 = sq[:].rearrange("p (h i two) -> p h i two", i=64, two=2)
                            r4 = rr[:].rearrange("p (h i two) -> p h i two", i=64, two=2)
                            xe, xo = s4[:, :, :, 0], s4[:, :, :, 1]
                            cb = bc_ap(rope[:, 0, :], [(0, 10), (1, 64)])
                            sbb = bc_ap(rope[:, 1, :], [(0, 10), (1, 64)])
                            tk.op("dve", [bsq, brope], [brr], "tensor_tensor", out=ra[:], in0=xe, in1=cb, op=ALU.mult)
                            tk.op("dve", [bsq, brope], [brr], "tensor_tensor", out=rb[:], in0=xo, in1=sbb, op=ALU.mult)
                            tk.op("dve", [brr], [brr], "tensor_tensor", out=r4[:, :, :, 0], in0=ra[:], in1=rb[:], op=ALU.subtract)
                            tk.op("dve", [bsq, brope, brr], [brr], "tensor_tensor", out=ra[:], in0=xe, in1=sbb, op=ALU.mult)
                            tk.op("dve", [bsq, brope, brr], [brr], "tensor_tensor", out=rb[:], in0=xo, in1=cb, op=ALU.mult)
                            tk.op("dve", [brr], [brr], "tensor_tensor", out=r4[:, :, :, 1], in0=ra[:], in1=rb[:], op=ALU.add)
                            src = rr
                            bsrc = brr
                        else:
                            src = sq
                            bsrc = bsq
                        tk.op("act", [bsrc], [bqkb], "activation", out=qkb[:, 0:1024], in_=src[:, 0:1024], func=AF.Copy, scale=128 ** -0.5)
                        tk.op("act", [bsrc], [bqkb], "activation", out=qkb[:, 1024:1280], in_=src[:, 1024:1280], func=AF.Copy)
                        tk.op("act", [bp], [bvb], "activation", out=vb[:], in_=p_sb[:, 1280:1536], func=AF.Copy)
                        for hh in range(10):
                            tk.op("pe", [bqkb, bC], [bpT2], "transpose", out=pT2[:, hh, :], in_=qkb[:, hh * 128:(hh + 1) * 128], identity=identb[:])
                        tk.op("dve", [bpT2], [bqkT], "tensor_copy", out=qkT[:], in_=pT2[:])
                        tk.dma("pool", [bqkT], [bAQ], AQT[b, :, :, t * 128:(t + 1) * 128], qkT[:, 0:8, :])
                        tk.dma("pool", [bqkT], [bAK], AKT[b, :, :, t * 128:(t + 1) * 128], qkT[:, 8:10, :])
                        tk.dma("pool", [bvb], [bAV], AVV[b, t * 128:(t + 1) * 128], vb[:].rearrange("p (g d) -> p g d", d=128))
                tk.barrier()
            with contextlib.ExitStack() as es:
                gq, bgq = load_bc(es, "at_gq", att_qn_g[ai:ai + 1, :])
                gk, bgk = load_bc(es, "at_gk", att_kn_g[ai:ai + 1, :])
                mm_ = sb(es, "at_mm", [128, 4])
                bmm = Buf()
                tk.op("dve", [bgq], [bmm], "tensor_reduce", out=mm_[:, 0:1], in_=gq[:], axis=AX.X, op=ALU.max, apply_absolute_value=True)
                tk.op("dve", [bgk], [bmm], "tensor_reduce", out=mm_[:, 1:2], in_=gk[:], axis=AX.X, op=ALU.max, apply_absolute_value=True)
                tk.op("dve", [bmm], [bmm], "scalar_tensor_tensor", out=mm_[:, 2:3], in0=mm_[:, 0:1], scalar=-(128 ** 0.5), in1=mm_[:, 1:2],
                      op0=ALU.mult, op1=ALU.mult)
                KT = sb(es, "at_KT", [128, NT], BF16)
                Va = sb(es, "at_Va", [128, NTT, 129], BF16)
                bKT, bVa = Buf(), Buf()
                QB = min(512, T)
                QTt = [sb(es, "at_QT%d" % i, [128, 512], BF16) for i in range(2)]
                bQT = [Buf(), Buf()]
                pS = [ps(es, "at_pS%d" % i, [128, 512]) for i in range(2)]
                bpS = [Buf(), Buf()]
                Pt = [sb(es, "at_P%d" % i, [128, 512], BF16) for i in range(2)]
                bP = [Buf(), Buf()]
                pO = [ps(es, "at_pO%d" % i, [128, 512]) for i in range(4)]
                bpO = [Buf() for _ in range(4)]
                osb = [sb(es, "at_o%d" % i, [128, 128]) for i in range(2)]
                rd = [sb(es, "at_rd%d" % i, [128, 1]) for i in range(2)]
                bo = [Buf(), Buf()]
                nq = 0
                no = 0
                for b in range(NB):
                    for g in range(2):
                        tk.dma("sp", [bAK], [bKT], KT[:], AKT[b, :, g, :])
                        tk.op("dve", [], [bVa], "memset", Va[:, :, 128:129], 1.0)
                        tk.dma("sp", [bAV], [bVa], Va[:, :, 0:128], AVV[b, :, g, :].rearrange("(c p) d -> p c d", p=128))
                        blocks = [(CL + qb * QB, QB, NTT) for qb in range(T // QB)]
                        if not last:
                            blocks.append((0, CL, CT))
                        for h in range(4):
                            for (q0, qn, nkc) in blocks:
                                i = nq % 2
                                nq += 1
                                tk.dma("sp", [bAQ], [bQT[i]], QTt[i][:, 0:qn], AQT[b, :, g * 4 + h, q0:q0 + qn])
                                nqs = qn // 128
                                for kc in range(nkc):
                                    j = kc % 2
                                    tk.op("pe", [bKT, bQT[i]], [bpS[j]], "matmul", pS[j][:, 0:qn], lhsT=KT[:, kc * 128:(kc + 1) * 128],
                                          rhs=QTt[i][:, 0:qn], start=True, stop=True)
                                    tk.op("act", [bpS[j], bmm], [bP[j]], "activation", out=Pt[j][:, 0:qn], in_=pS[j][:, 0:qn], func=AF.Exp,
                                          bias=mm_[:, 2:3])
                                    for qs in range(nqs):
                                        tk.op("pe", [bP[j], bVa], [bpO[qs]], "matmul", pO[qs][:, 0:129], lhsT=Pt[j][:, qs * 128:(qs + 1) * 128],
                                              rhs=Va[:, kc, :], start=(kc == 0), stop=(kc == nkc - 1))
                                for qs in range(nqs):
                                    o_ = no % 2
                                    no += 1
                                    tk.op("dve", [bpO[qs]], [bo[o_]], "reciprocal", out=rd[o_][:], in_=pO[qs][:, 128:129])
                                    tk.op("dve", [bpO[qs], bo[o_]], [bo[o_]], "tensor_scalar", out=osb[o_][:], in0=pO[qs][:, 0:128],
                                          scalar1=rd[o_][:, 0:1], scalar2=None, op0=ALU.mult)
                                    r0 = q0 + qs * 128
                                    tk.dma("pool", [bo[o_]], [bAO], AO[b, r0:r0 + 128, (g * 4 + h) * 128:(g * 4 + h + 1) * 128], osb[o_][:])
                tk.barrier()
            with contextlib.ExitStack() as es:
                Wo = sb(es, "at_Wo", [128, 8, D], BF16)
                bWo = Buf()
                load_w_bf16(es, "ato", Wo, bWo, att_w_out[ai], D, 8)
                ao = sb(es, "at_ao", [128, D])
                aob = sb(es, "at_aob", [128, D], BF16)
                bao, baob = Buf(), Buf()
                xt = sb(es, "at_x", [128, D])
                bx = Buf()
                pT = ps(es, "at_pTc", [128, 8, 128], BF16)
                bpT = Buf()
                oT = sb(es, "at_oT", [128, 8, 128], BF16)
                boT = Buf()
                pY = ps(es, "at_pY", [128, D])
                bpY = Buf()
                yt = sb(es, "at_y", [128, D])
                by = Buf()
                g1t = sb(es, "at_g1", [128, D])
                bg1 = Buf()
                cur_n = None
                for b in range(NB):
                    for (t, n, isc) in seg_tiles(b):
                        if isc and last:
                            continue
                        if n != cur_n:
                            cur_n = n
                            tk.dma("sp", [bMOD], [bg1], g1t[:], MOD[l, n:n + 1, 2 * D:3 * D].to_broadcast([128, D]))
                        tk.dma("sp", [bAO], [bao], ao[:], AO[b, t * 128:(t + 1) * 128, :])
                        tk.dma("sp", [bXS[b][t]], [bx], xt[:], XS[b, t * 128:(t + 1) * 128, :])
                        tk.op("act", [bao], [baob], "activation", out=aob[:], in_=ao[:], func=AF.Copy)
                        for k in range(8):
                            tk.op("pe", [baob, bC], [bpT], "transpose", out=pT[:, k, :], in_=aob[:, k * 128:(k + 1) * 128], identity=identb[:])
                        tk.op("dve", [bpT], [boT], "tensor_copy", out=oT[:], in_=pT[:])
                        for cg in range(2):
                            for k in range(8):
                                tk.op("pe", [boT, bWo], [bpY], "matmul", pY[:, cg * 512:(cg + 1) * 512], lhsT=oT[:, k, :],
                                      rhs=Wo[:, k, cg * 512:(cg + 1) * 512], start=(k == 0), stop=(k == 7))
                        tk.op("dve", [bpY, bg1], [by], "tensor_tensor", out=yt[:], in0=pY[:], in1=g1t[:], op=ALU.mult)
                        tk.op("dve", [by, bx], [by], "tensor_tensor", out=yt[:], in0=yt[:], in1=xt[:], op=ALU.add)
                        tk.dma("pool", [by], [bXS[b][t]], XS[b, t * 128:(t + 1) * 128, :], yt[:])
                tk.barrier()

        def final_phase():
            with contextlib.ExitStack() as es:
                xt = [sb(es, "fn_x%d" % i, [128, D]) for i in range(2)]
                bx = [Buf(), Buf()]
                junk = sb(es, "fn_junk", [128, D])
                st = sb(es, "fn_st", [128, 4])
                bj, bst = Buf(), Buf()
                gb_, bgb = load_bc(es, "fn_g", final_g[0:1, :])
                yt = [sb(es, "fn_y%d" % i, [128, D]) for i in range(2)]
                by = [Buf(), Buf()]
                n = 0
                for b in range(NB):
                    for t in range(CL // 128, NT // 128):
                        i = n % 2
                        n += 1
                        tk.dma("sp", [bXS[b][t]], [bx[i]], xt[i][:], (XS if BIS != -2 else xin)[b, t * 128:(t + 1) * 128, :])
                        tk.op("dve", [bx[i]], [bj, bst], "scalar_tensor_tensor", out=junk[:], in0=xt[i][:], scalar=1.0, in1=xt[i][:],
                              op0=ALU.mult, op1=ALU.mult, accum_out=st[:, 0:1])
                        tk.op("act", [bst], [bst], "activation", out=st[:, 1:2], in_=st[:, 0:1], func=AF.Sqrt, bias=EPS,
                              scale=1.0 / D)
                        tk.op("dve", [bst], [bst], "reciprocal", out=st[:, 2:3], in_=st[:, 1:2])
                        tk.op("dve", [bx[i], bst, bgb], [by[i]], "scalar_tensor_tensor", out=yt[i][:], in0=xt[i][:],
                              scalar=st[:, 2:3], in1=gb_[:], op0=ALU.mult, op1=ALU.mult)
                        tl = t - CL // 128
                        tk.dma("pool", [by[i]], [bOUT], yout[b, tl * 128:(tl + 1) * 128, :], yt[i][:])
                tk.barrier()

        dn_i = att_i = 0
        for l, m in enumerate(cfg.layers):
            if m == "dn":
                dn_phase(l, dn_i)
                dn_i += 1
            elif m == "att":
                att_phase(l, att_i)
                att_i += 1
            if cfg.peer:
                peer_phase(l)
        if not os.environ.get("PEERDBG"):
            final_phase()
        tk.barrier()
        print("ninst", tk.ninst, flush=True)
    nc._in_names = in_names
    return nc


def rope_tables(T):
    rows = T // 64
    row = np.broadcast_to(np.arange(rows)[:, None], (rows, 64)).reshape(-1).astype(np.float32)
    col = np.broadcast_to(np.arange(64)[None, :], (rows, 64)).reshape(-1).astype(np.float32)
    freqs = (np.float32(10000.0) ** (-np.arange(0, 64, 2, dtype=np.float32) / np.float32(64))).astype(np.float32)
    ang = np.concatenate([row[:, None] * freqs, col[:, None] * freqs], axis=-1).astype(np.float32)
    return np.stack([np.cos(ang), np.sin(ang)], axis=1).astype(np.float32)


def const_inputs(T):
    i = np.arange(64)
    cum_f = (i[:, None] <= i[None, :]).astype(np.float32)
    incl_f = cum_f.copy()
    strict_f = (i[:, None] < i[None, :]).astype(np.float32)
    cum_b = cum_f.T.copy()
    incl_b = incl_f.T.copy()
    strict_b = strict_f.T.copy()
    masks = np.stack([cum_f, incl_f, strict_f, cum_b, incl_b, strict_b], axis=1).astype(np.float32)
    return {
        "c_ident": np.eye(128, dtype=np.float32),
        "c_masks": masks,
        "c_rope": rope_tables(T),
        "c_iota": np.broadcast_to(np.arange(16, dtype=np.float32)[None, :], (128, 16)).copy(),
    }


def make_in_maps(cfg, inputs, ncores, names=None):
    NB = cfg.NB
    f = lambda a: np.ascontiguousarray(np.asarray(a, dtype=np.float32))
    consts = const_inputs(cfg.T)
    shared = {k: f(inputs[k]) for k in ("ada_w", "ada_b", "norm1_g", "norm2_g", "dn_w_in", "dn_conv_w", "dn_norm_g",
                                         "dn_w_out", "att_w_in", "att_qn_g", "att_kn_g", "att_w_out", "peer_w_query",
                                         "peer_sub_keys", "peer_u", "peer_v")}
    shared["final_g"] = f(inputs["final_g"]).reshape(1, D)
    shared["dn_a_log"] = f(inputs["dn_a_log"]).reshape(-1, 32)
    shared["dn_dt_bias"] = f(inputs["dn_dt_bias"]).reshape(-1, 32)
    shared.update(consts)
    maps = []
    for c in range(ncores):
        bs = slice(c * NB, (c + 1) * NB)
        m = dict(shared)
        m["xin"] = np.ascontiguousarray(np.concatenate([f(inputs["ctx"])[bs], f(inputs["x"])[bs]], axis=1))
        cnd = np.zeros((3, D), np.float32)
        cnd[0:NB] = f(inputs["c"])[bs]
        cnd[2] = f(inputs["c_ctx"])
        m["cnd"] = cnd
        if names is not None:
            m = {k: v for k, v in m.items() if k in names}
        maps.append(m)
    return maps


def kernel(**inputs):
    cfg = Cfg()
    nc = build_program(cfg)
    maps = make_in_maps(cfg, inputs, NCORES, nc._in_names)
    res = run_bass_kernel_spmd(nc, maps, core_ids=list(range(NCORES)))
    return np.concatenate([r["yout"] for r in res.results], axis=0).astype(np.float32)
```
